# Optimizing a Trainium2 kernel written in Bass

```python
import jax, jax.numpy as jnp
from jax import lax
import numpy as np

D_MODEL = 1024
BATCH = 32
SEQ = 2048
DEPTH = 1

HEAD_DIM = 64
ROPE_THETA = 10000.0
A_Q_HEADS = 16
A_KV_HEADS = 4
A_GQA = A_Q_HEADS // A_KV_HEADS
A_WINDOW = 128
B_GROUPS = ((128, 1), (512, 4), (2048, 16))
B_HEADS_PER_GROUP = 4
ATTN_BLOCK = 128
A_Q_W = A_Q_HEADS * HEAD_DIM
A_KV_W = A_KV_HEADS * HEAD_DIM
B_OUT_W = B_HEADS_PER_GROUP * HEAD_DIM
B_W = len(B_GROUPS) * B_OUT_W
N_BRANCH = 2
OFF_AQ = 0
OFF_AK = OFF_AQ + A_Q_W
OFF_AV = OFF_AK + A_KV_W
OFF_BQ = OFF_AV + A_KV_W
OFF_BK = OFF_BQ + B_W
OFF_BV = OFF_BK + B_W
OFF_G = OFF_BV + B_W
IN_W = OFF_G + N_BRANCH * D_MODEL
N_EXPERTS = 32
TOP_K = 4
D_EXPERT = D_MODEL
SWIGLU_LIMIT = 7.0
SWIGLU_ALPHA = 1.702
MOE_BLOCK = 512
LN_EPS = 1e-5
DEEPNORM_ALPHA = (2 * DEPTH) ** 0.25
DEEPNORM_BETA = (8 * DEPTH) ** -0.25

kernel_name = 'hybrid_swa_sink_dilated_moe_deepnorm'


def layer_norm(x, g, b):
    xf = x.astype(jnp.float32)
    mu = jnp.mean(xf, axis=-1, keepdims=True)
    var = jnp.mean(jnp.square(xf - mu), axis=-1, keepdims=True)
    y = (xf - mu) * lax.rsqrt(var + LN_EPS)
    return (y * g.astype(jnp.float32) + b.astype(jnp.float32)).astype(x.dtype)


def rope(x, pos):
    half = x.shape[-1] // 2
    inv_freq = ROPE_THETA ** (-jnp.arange(0, x.shape[-1], 2, dtype=jnp.float32) / x.shape[-1])
    ang = pos[:, None] * inv_freq[None, :]
    cos = jnp.cos(ang)[None, :, None, :]
    sin = jnp.sin(ang)[None, :, None, :]
    xf = x.astype(jnp.float32)
    x1, x2 = xf[..., :half], xf[..., half:]
    return jnp.concatenate([x1 * cos - x2 * sin, x2 * cos + x1 * sin], axis=-1).astype(x.dtype)


def banded_attention(q, k, v, n_back, sink):
    n, L, hk, g, hd = q.shape
    blk = ATTN_BLOCK
    nb = -(-L // blk)
    pad = nb * blk - L
    if pad:
        q = jnp.pad(q, ((0, 0), (0, pad), (0, 0), (0, 0), (0, 0)))
        k = jnp.pad(k, ((0, 0), (0, pad), (0, 0), (0, 0)))
        v = jnp.pad(v, ((0, 0), (0, pad), (0, 0), (0, 0)))
    qb = q.reshape(n, nb, blk, hk, g, hd)

    def with_prev(t):
        tb = t.reshape(n, nb, blk, hk, hd)
        prev = jnp.pad(tb, ((0, 0), (1, 0), (0, 0), (0, 0), (0, 0)))[:, :-1]
        return jnp.concatenate([prev, tb], axis=2)

    kk, vv = with_prev(k), with_prev(v)
    s = jnp.einsum('nbqhgd,nbkhd->nbhgqk', qb, kk,
                   preferred_element_type=jnp.float32) * (hd ** -0.5)
    qi = jnp.arange(blk)[:, None] + blk
    kj = jnp.arange(2 * blk)[None, :]
    dist = qi - kj
    band = (dist >= 0) & (dist <= n_back)
    key_pos = jnp.arange(nb)[:, None, None] * blk + kj[None] - blk
    valid = band[None] & (key_pos >= 0)
    s = jnp.where(valid[None, :, None, None], s, -jnp.inf)
    m = jnp.max(s, axis=-1)
    if sink is not None:
        sk = sink.astype(jnp.float32)[:, :, None]
        m = jnp.maximum(m, sk)
    p = jnp.exp(s - m[..., None])
    l = jnp.sum(p, axis=-1)
    if sink is not None:
        l = l + jnp.exp(sk - m)
    o = jnp.einsum('nbhgqk,nbkhd->nbqhgd', p.astype(v.dtype), vv,
                   preferred_element_type=jnp.float32)
    l_t = jnp.moveaxis(l, -1, 2)
    o = o / l_t[..., None]
    lse = jnp.moveaxis(m, -1, 2) + jnp.log(l_t)
    o = o.reshape(n, nb * blk, hk, g, hd)[:, :L]
    lse = lse.reshape(n, nb * blk, hk, g)[:, :L]
    return o.astype(q.dtype), lse


def to_class(t, d):
    b, s = t.shape[:2]
    t = t.reshape(b, s // d, d, *t.shape[2:])
    t = jnp.moveaxis(t, 2, 1)
    return t.reshape(b * d, s // d, *t.shape[3:])


def from_class(t, d, b):
    t = t.reshape(b, d, t.shape[1], *t.shape[2:])
    t = jnp.moveaxis(t, 1, 2)
    return t.reshape(b, -1, *t.shape[3:])


def hybrid_mixer(x, w_in, sinks, w_branch_a, w_branch_b, w_out):
    b, s, _ = x.shape
    pos = jnp.arange(s, dtype=jnp.float32)
    proj = jnp.einsum('bsd,de->bse', x, w_in)

    def heads(lo, w):
        return proj[..., lo:lo + w].reshape(b, s, w // HEAD_DIM, HEAD_DIM)

    qa = rope(heads(OFF_AQ, A_Q_W), pos).reshape(b, s, A_KV_HEADS, A_GQA, HEAD_DIM)
    ka = rope(heads(OFF_AK, A_KV_W), pos)
    va = heads(OFF_AV, A_KV_W)
    oa, _ = banded_attention(qa, ka, va, A_WINDOW - 1, sinks.reshape(A_KV_HEADS, A_GQA))
    oa = oa.reshape(b, s, A_Q_W)

    qb = rope(heads(OFF_BQ, B_W), pos)
    kb = rope(heads(OFF_BK, B_W), pos)
    vb = heads(OFF_BV, B_W)
    outs, lses = [], []
    for gi, (window, dil) in enumerate(B_GROUPS):
        sl = slice(gi * B_HEADS_PER_GROUP, (gi + 1) * B_HEADS_PER_GROUP)
        qc = to_class(qb[:, :, sl], dil)[:, :, :, None]
        kc = to_class(kb[:, :, sl], dil)
        vc = to_class(vb[:, :, sl], dil)
        o, lse = banded_attention(qc, kc, vc, window // dil, None)
        outs.append(from_class(o[:, :, :, 0], dil, b))
        lses.append(from_class(lse[:, :, :, 0], dil, b))
    wts = jax.nn.softmax(jnp.stack(lses, axis=0), axis=0)
    ob = jnp.sum(wts[..., None] * jnp.stack(outs, axis=0).astype(jnp.float32), axis=0)
    ob = ob.astype(x.dtype).reshape(b, s, B_OUT_W)

    gates = jax.nn.sigmoid(proj[..., OFF_G:].reshape(b, s, N_BRANCH, D_MODEL)
                           .astype(jnp.float32)).astype(x.dtype)
    ya = jnp.einsum('bsi,id->bsd', oa, w_branch_a)
    yb = jnp.einsum('bsi,id->bsd', ob, w_branch_b)
    merged = gates[:, :, 0] * ya + gates[:, :, 1] * yb
    return jnp.einsum('bsd,de->bse', merged, w_out)


def moe_ffn(x, w_router, b_router, w_gate_up, b_gate_up, w_down, b_down):
    b, s, d = x.shape
    t = b * s
    xt = x.reshape(t, d)
    logits = jnp.einsum('td,de->te', xt, w_router,
                        preferred_element_type=jnp.float32) + b_router.astype(jnp.float32)
    top_v, top_e = lax.top_k(logits, TOP_K)
    gate_w = jax.nn.softmax(top_v, axis=-1)
    n = t * TOP_K
    flat_e = top_e.reshape(n)
    flat_w = gate_w.reshape(n)
    order = jnp.argsort(flat_e)
    sorted_e = flat_e[order]
    sorted_tok = (order // TOP_K).astype(jnp.int32)
    sorted_w = flat_w[order]
    counts = jnp.bincount(flat_e, length=N_EXPERTS)
    padded = (counts + MOE_BLOCK - 1) // MOE_BLOCK * MOE_BLOCK
    pad_end = jnp.cumsum(padded)
    pad_start = pad_end - padded
    start = jnp.cumsum(counts) - counts
    dest = pad_start[sorted_e] + (jnp.arange(n) - start[sorted_e])
    nblk = -(-(n + N_EXPERTS * (MOE_BLOCK - 1)) // MOE_BLOCK)
    rows = nblk * MOE_BLOCK
    buf_tok = jnp.full((rows,), t, jnp.int32).at[dest].set(sorted_tok)
    buf_w = jnp.zeros((rows,), jnp.float32).at[dest].set(sorted_w)
    blk_e = jnp.minimum(jnp.searchsorted(pad_end, jnp.arange(nblk) * MOE_BLOCK, side='right'),
                        N_EXPERTS - 1)
    x_pad = jnp.concatenate([xt, jnp.zeros((1, d), xt.dtype)], axis=0)

    def expert_block(args):
        tok, w, e = args
        xb = x_pad[tok]
        gu = xb @ w_gate_up[e] + b_gate_up[e]
        gate = jnp.minimum(gu[:, :D_EXPERT], SWIGLU_LIMIT)
        up = jnp.clip(gu[:, D_EXPERT:], -SWIGLU_LIMIT, SWIGLU_LIMIT)
        h = (up + 1.0) * gate * jax.nn.sigmoid(SWIGLU_ALPHA * gate)
        y = h @ w_down[e] + b_down[e]
        return y * w[:, None].astype(y.dtype)

    y = lax.map(expert_block, (buf_tok.reshape(nblk, MOE_BLOCK),
                               buf_w.reshape(nblk, MOE_BLOCK), blk_e))
    out = jnp.zeros((t + 1, d), y.dtype).at[buf_tok].add(y.reshape(rows, d))[:t]
    return out.reshape(b, s, d)


def setup_inputs(seed: int = 0) -> dict:
    key = jax.random.key(seed)
    ks = jax.random.split(key, 16)
    f32 = jnp.float32

    def nrm(k, shape):
        return jax.random.normal(k, shape, f32)

    col_scale = jnp.concatenate([
        jnp.ones((A_Q_W + A_KV_W,), f32), jnp.full((A_KV_W,), DEEPNORM_BETA, f32),
        jnp.ones((2 * B_W,), f32), jnp.full((B_W,), DEEPNORM_BETA, f32),
        jnp.ones((N_BRANCH * D_MODEL,), f32)])
    return {
        'x': nrm(ks[0], (BATCH, SEQ, D_MODEL)),
        'w_in': nrm(ks[1], (DEPTH, D_MODEL, IN_W)) * (D_MODEL ** -0.5) * col_scale,
        'attn_sinks': 0.5 * nrm(ks[2], (DEPTH, A_Q_HEADS)),
        'w_branch_a': nrm(ks[3], (DEPTH, A_Q_W, D_MODEL)) * (A_Q_W ** -0.5),
        'w_branch_b': nrm(ks[4], (DEPTH, B_OUT_W, D_MODEL)) * (B_OUT_W ** -0.5),
        'w_out': nrm(ks[5], (DEPTH, D_MODEL, D_MODEL)) * (D_MODEL ** -0.5) * DEEPNORM_BETA,
        'ln1_g': 1.0 + 0.02 * nrm(ks[6], (DEPTH, D_MODEL)),
        'ln1_b': 0.02 * nrm(ks[7], (DEPTH, D_MODEL)),
        'w_router': nrm(ks[8], (DEPTH, D_MODEL, N_EXPERTS)) * (D_MODEL ** -0.5),
        'b_router': 0.01 * nrm(ks[9], (DEPTH, N_EXPERTS)),
        'w_gate_up': nrm(ks[10], (DEPTH, N_EXPERTS, D_MODEL, 2 * D_EXPERT)) * (D_MODEL ** -0.5),
        'b_gate_up': 0.02 * nrm(ks[11], (DEPTH, N_EXPERTS, 2 * D_EXPERT)),
        'w_down': nrm(ks[12], (DEPTH, N_EXPERTS, D_EXPERT, D_MODEL)) * (D_EXPERT ** -0.5) * DEEPNORM_BETA,
        'b_down': 0.02 * nrm(ks[13], (DEPTH, N_EXPERTS, D_MODEL)),
        'ln2_g': 1.0 + 0.02 * nrm(ks[14], (DEPTH, D_MODEL)),
        'ln2_b': 0.02 * nrm(ks[15], (DEPTH, D_MODEL)),
    }


def reference(x, w_in, attn_sinks, w_branch_a, w_branch_b, w_out, ln1_g, ln1_b,
              w_router, b_router, w_gate_up, b_gate_up, w_down, b_down, ln2_g, ln2_b):
    h = x
    for l in range(DEPTH):
        mix = hybrid_mixer(h, w_in[l], attn_sinks[l], w_branch_a[l], w_branch_b[l], w_out[l])
        h = layer_norm(DEEPNORM_ALPHA * h + mix, ln1_g[l], ln1_b[l])
        ffn = moe_ffn(h, w_router[l], b_router[l], w_gate_up[l], b_gate_up[l], w_down[l], b_down[l])
        h = layer_norm(DEEPNORM_ALPHA * h + ffn, ln2_g[l], ln2_b[l])
    return h
```

```python
import os
import numpy as np
from contextlib import ExitStack
import concourse.bass as bass
import concourse.mybir as mybir
from concourse.bass_utils import run_bass_kernel_spmd

F32 = mybir.dt.float32
BF16 = mybir.dt.bfloat16
I32 = mybir.dt.int32
AF = mybir.ActivationFunctionType
ALU = mybir.AluOpType

D = 1024
SEQ = 2048
NEXP = 32
ALPHA = 2.0 ** 0.25
EPS = 1e-5
NQK = 22
T_AQ = 0
T_AK = 8
T_BQ = 0
T_BK = 6
B_GROUPS = ((128, 1), (512, 4), (2048, 16))


class Eng:
    def __init__(self, nc, eng, name, es):
        self.e = eng
        self.name = name
        self.sem = es.enter_context(nc.semaphore("s_" + name))
        self.count = 0
        self.seen = {}

    def wait(self, tok):
        if tok is None:
            return
        sem, v, key = tok
        if self.seen.get(key, 0) >= v:
            return
        self.seen[key] = v
        self.e.wait_ge(sem, v)

    def issue(self, ins):
        ins.then_inc(self.sem, 1)
        self.count += 1
        return (self.sem, self.count, self.name)

    def alltoks(self):
        return [(self.sem, self.count, self.name)] if self.count else []


class DmaQ:
    def __init__(self, nc, eng, name, es, nsems=8):
        self.e = eng
        self.name = name
        self.sems = [es.enter_context(nc.semaphore("d_%s%d" % (name, i))) for i in range(nsems)]
        self.counts = [0] * nsems
        self.n = 0
        self.seen = {}

    def wait(self, tok):
        if tok is None:
            return
        sem, v, key = tok
        if self.seen.get(key, 0) >= v:
            return
        self.seen[key] = v
        self.e.wait_ge(sem, v)

    def issue(self, fn):
        i = self.n % len(self.sems)
        self.n += 1
        key = "%s_%d" % (self.name, i)
        if self.counts[i] > 0:
            self.wait((self.sems[i], self.counts[i], key))
        ins = fn()
        ins.then_inc(self.sems[i], 16)
        self.counts[i] += 16
        return (self.sems[i], self.counts[i], key)

    def alltoks(self):
        return [(s, c, "%s_%d" % (self.name, i)) for i, (s, c) in enumerate(zip(self.sems, self.counts)) if c]


class T:
    def __init__(self, ap=None, name=""):
        self.ap = ap
        self.name = name
        self.w = None
        self.r = {}


def _deps(q, reads, writes):
    for t in reads:
        q.wait(t.w)
    for t in writes:
        q.wait(t.w)
        for r in t.r.values():
            q.wait(r)


def _commit(tok, reads, writes):
    for t in reads:
        t.r[tok[2]] = tok
    for t in writes:
        t.w = tok
        t.r = {}


def op(q, fn, reads=(), writes=()):
    _deps(q, reads, writes)
    tok = q.issue(fn())
    _commit(tok, reads, writes)
    return tok


def mm(q, fns, reads=(), writes=()):
    _deps(q, reads, writes)
    ins = None
    for f in fns:
        ins = f()
    tok = q.issue(ins)
    _commit(tok, reads, writes)
    return tok


def dma(q, fn, reads=(), writes=()):
    _deps(q, reads, writes)
    tok = q.issue(fn)
    _commit(tok, reads, writes)
    return tok


def build(NSEQ=4, C=1280, debug=False, stop_after=5):
    NT = NSEQ * SEQ
    NCH = NT // 512
    NTT = NT // 128
    NSLOT = NEXP * C
    nc = bass.Bass("TRN2", target_bir_lowering=False)

    def din(name, shape, dt=F32):
        return nc.dram_tensor(name, shape, dt, kind="ExternalInput").ap()

    def dscr(name, shape, dt):
        return nc.dram_tensor(name, shape, dt, kind=("ExternalOutput" if debug else "Internal")).ap()

    x = din("x", [NT, D])
    w_inp = din("w_inp", [D, 5888])
    w_a = din("w_a", [D, D])
    w_b = din("w_b", [256, D])
    w_o = din("w_o", [D, D])
    sinks = din("sinks", [1, 16])
    ln1_g = din("ln1_g", [1, D]); ln1_b = din("ln1_b", [1, D])
    ln2_g = din("ln2_g", [1, D]); ln2_b = din("ln2_b", [1, D])
    w_r = din("w_r", [D, NEXP]); b_r = din("b_r", [1, NEXP])
    NEW = NEXP if stop_after >= 4 else 1
    w_gu = din("w_gu", [NEW, D, 2 * D]); b_gu = din("b_gu", [NEXP, 2 * D])
    w_d = din("w_d", [NEW, D, D]); b_d = din("b_d", [NEXP, D])
    cos_t = din("cos_t", [128, SEQ]); sin_t = din("sin_t", [128, SEQ])
    masks_in = din("masks", [128, 3, 512])
    hm_in = din("hm", [128, 4])
    tri_in = din("tri", [128, 128]); ident_in = din("ident", [128, 128]); ec_in = din("ec", [128, NEXP])
    out = nc.dram_tensor("out", [NT, D], F32, kind="ExternalOutput").ap()

    xb = dscr("xb", [NT, D], BF16)
    qk_scr = dscr("qk_scr", [NQK, 128, NT], BF16)
    v_scr = dscr("v_scr", [NT, 16, 128], BF16)
    oT_scr = dscr("oT_scr", [1280, NT], BF16)
    h1_scr = dscr("h1_scr", [NT, D], F32)
    xg_scr = dscr("xg_scr", [NSLOT, D], BF16)
    y_scr = dscr("y_scr", [NSLOT, D], F32)

    with ExitStack() as es:
        pe = Eng(nc, nc.tensor, "pe", es)
        act = Eng(nc, nc.scalar, "act", es)
        dve = Eng(nc, nc.vector, "dve", es)
        pool = Eng(nc, nc.gpsimd, "pool", es)
        sq = DmaQ(nc, nc.sync, "sq", es, 8)
        gq = DmaQ(nc, nc.gpsimd, "gq", es, 8)
        allq = [pe, act, dve, pool, sq, gq]

        def barrier():
            toks = []
            for q in allq:
                toks += q.alltoks()
            for q in allq:
                for t in toks:
                    q.wait(t)

        def sbt(st, name, shape, dt):
            return T(st.enter_context(nc.sbuf_tensor("sb_" + name, shape, dt)), name)

        def pst(st, name, shape, dt=F32):
            return T(st.enter_context(nc.psum_tensor("ps_" + name, shape, dt)), name)

        bc_reg = nc.gpsimd.to_reg(NSLOT - 1)
        XB = T(xb); QK = T(qk_scr); VS = T(v_scr); OT = T(oT_scr); H1 = T(h1_scr); XG = T(xg_scr); YS = T(y_scr)

        cst = es
        ident = sbt(cst, "ident", [128, 128], F32)
        tri = sbt(cst, "tri", [128, 128], BF16)
        onesb = sbt(cst, "onesb", [128, 128], BF16)
        ec = sbt(cst, "ec", [128, NEXP], F32)
        slots_all = sbt(cst, "slots_all", [128, NTT, 4], I32)
        wts_all = sbt(cst, "wts_all", [128, NTT, 4], F32)
        dma(sq, lambda: nc.sync.dma_start(out=ident.ap[:, :], in_=ident_in[:, :]), writes=[ident])
        dma(gq, lambda: nc.gpsimd.dma_start(out=tri.ap[:, :], in_=tri_in[:, :]), writes=[tri])
        dma(sq, lambda: nc.sync.dma_start(out=ec.ap[:, :], in_=ec_in[:, :]), writes=[ec])
        op(pool, lambda: nc.gpsimd.memset(onesb.ap[:, :], 1.0), writes=[onesb])

        XBs = [T(xb) for _ in range(NT // 1024)]

        def cast_x():
            for i in range(NT // 1024):
                dma(gq, lambda i=i: nc.gpsimd.dma_start(out=xb[i * 1024:(i + 1) * 1024, :], in_=x[i * 1024:(i + 1) * 1024, :]), writes=[XBs[i]])

        with ExitStack() as p1:
            wqkv = sbt(p1, "wqkv", [128, 8, 3840], BF16)
            cosb = sbt(p1, "cosb", [128, SEQ], F32)
            sinb = sbt(p1, "sinb", [128, SEQ], F32)
            xT = [sbt(p1, "xT%d" % i, [128, 8, 512], BF16) for i in range(2)]
            pA = [pst(p1, "pA%d" % i, [128, 512]) for i in range(2)]
            pB = [pst(p1, "pB%d" % i, [128, 512]) for i in range(2)]
            pV = [pst(p1, "pV%d" % i, [128, 512]) for i in range(2)]
            t1 = [sbt(p1, "t1_%d" % i, [128, 512], F32) for i in range(2)]
            t2 = [sbt(p1, "t2_%d" % i, [128, 512], F32) for i in range(2)]
            t3 = [sbt(p1, "t3_%d" % i, [128, 512], F32) for i in range(2)]
            t4 = [sbt(p1, "t4_%d" % i, [128, 512], F32) for i in range(2)]
            qo = [sbt(p1, "qo%d" % i, [128, 2, 512], BF16) for i in range(2)]
            vsb = [sbt(p1, "vsb%d" % i, [128, 16, 128], BF16) for i in range(2)]
            wv = w_inp.rearrange("(c p) n -> p c n", p=128)
            for c0 in range(0, 3840, 1280):
                dma(gq, lambda c0=c0: nc.gpsimd.dma_start(out=wqkv.ap[:, :, c0:c0 + 1280], in_=wv[:, :, c0:c0 + 1280]), writes=[wqkv])
            cast_x()
            dma(sq, lambda: nc.sync.dma_start(out=cosb.ap[:, :], in_=cos_t[:, :]), writes=[cosb])
            dma(sq, lambda: nc.sync.dma_start(out=sinb.ap[:, :], in_=sin_t[:, :]), writes=[sinb])
            for i in range(2):
                op(pool, lambda i=i: nc.gpsimd.memset(vsb[i].ap[:, :, :], 1.0), writes=[vsb[i]])
            zt = sbt(p1, "zt", [128, 8, D], BF16)
            op(pool, lambda: nc.gpsimd.memset(zt.ap[:, :, :], 0.0), writes=[zt])
            for r0_ in range(0, NSLOT, 1024):
                n_ = min(1024, NSLOT - r0_) // 128
                dma(gq, lambda r0_=r0_, n_=n_: nc.gpsimd.dma_start(out=xg_scr[r0_:r0_ + 128 * n_, :].rearrange("(p k) d -> p k d", k=n_), in_=zt.ap[:, 0:n_, :]),
                    reads=[zt], writes=[XG])
            npair = 0
            nv = 0

            def load_xT(ch_):
                for c in range(8):
                    dma(sq, lambda c=c: nc.sync.dma_start_transpose(out=xT[ch_ % 2].ap[:, c, :], in_=xb[ch_ * 512:ch_ * 512 + 512, c * 128:(c + 1) * 128]),
                        reads=[XBs[ch_ // 2]], writes=[xT[ch_ % 2]])

            for ch in range(NCH):
                tok0 = ch * 512
                pos0 = tok0 % SEQ
                xt = xT[ch % 2]
                if ch == 0:
                    load_xT(0)
                if ch + 1 < NCH:
                    load_xT(ch + 1)
                for pr in range(NQK // 2):
                    a, b = pA[npair % 2], pB[npair % 2]
                    u1, u2, u3, u4, o = t1[npair % 2], t2[npair % 2], t3[npair % 2], t4[npair % 2], qo[npair % 2]
                    npair += 1
                    for (pt, ti) in ((a, 2 * pr), (b, 2 * pr + 1)):
                        mm(pe, [lambda c=c, pt=pt, ti=ti: nc.tensor.matmul(pt.ap[:, :], lhsT=wqkv.ap[:, c, ti * 128:(ti + 1) * 128], rhs=xt.ap[:, c, :],
                                                                           start=(c == 0), stop=(c == 7)) for c in range(8)],
                           reads=[wqkv, xt], writes=[pt])
                    cs = cosb.ap[:, pos0:pos0 + 512]
                    sn = sinb.ap[:, pos0:pos0 + 512]
                    op(dve, lambda: nc.vector.tensor_tensor(out=u1.ap[:, :], in0=a.ap[:, :], in1=cs, op=ALU.mult), reads=[a, cosb], writes=[u1])
                    op(dve, lambda: nc.vector.tensor_tensor(out=u2.ap[:, :], in0=b.ap[:, :], in1=sn, op=ALU.mult), reads=[b, sinb], writes=[u2])
                    op(dve, lambda: nc.vector.tensor_tensor(out=u3.ap[:, :], in0=b.ap[:, :], in1=cs, op=ALU.mult), reads=[b, cosb], writes=[u3])
                    op(dve, lambda: nc.vector.tensor_tensor(out=u4.ap[:, :], in0=a.ap[:, :], in1=sn, op=ALU.mult), reads=[a, sinb], writes=[u4])
                    op(pool, lambda: nc.gpsimd.tensor_tensor(out=o.ap[:, 0, :], in0=u1.ap[:, :], in1=u2.ap[:, :], op=ALU.subtract), reads=[u1, u2], writes=[o])
                    op(pool, lambda: nc.gpsimd.tensor_tensor(out=o.ap[:, 1, :], in0=u3.ap[:, :], in1=u4.ap[:, :], op=ALU.add), reads=[u3, u4], writes=[o])
                    dma(sq, lambda: nc.sync.dma_start(out=qk_scr[2 * pr:2 * pr + 2, :, tok0:tok0 + 512].rearrange("t p n -> p t n"), in_=o.ap[:, :, :]),
                        reads=[o], writes=[QK])
                for tt in range(4):
                    vb = vsb[nv % 2]
                    nv += 1
                    for hf in range(2):
                        pv = pV[hf]
                        mm(pe, [lambda c=c, pv=pv, hf=hf: nc.tensor.matmul(pv.ap[:, :], lhsT=xt.ap[:, c, tt * 128:(tt + 1) * 128],
                                                                           rhs=wqkv.ap[:, c, 2816 + hf * 512:2816 + (hf + 1) * 512],
                                                                           start=(c == 0), stop=(c == 7)) for c in range(8)],
                           reads=[wqkv, xt], writes=[pv])
                        op(act, lambda pv=pv, hf=hf, vb=vb: nc.scalar.copy(out=vb.ap[:, hf * 8:(hf + 1) * 8, 0:64],
                                                                           in_=pv.ap[:, :].rearrange("p (h d) -> p h d", d=64)),
                           reads=[pv], writes=[vb])
                    dma(sq, lambda vb=vb, tt=tt: nc.sync.dma_start(out=v_scr[tok0 + tt * 128:tok0 + (tt + 1) * 128, :, :], in_=vb.ap[:, :, :]),
                        reads=[vb], writes=[VS])
            barrier()
        if stop_after <= 1:
            return nc

        with ExitStack() as p2:
            qk = sbt(p2, "qk", [128, 12, SEQ], BF16)
            vA = sbt(p2, "vA", [128, 16, 4, 128], BF16)
            kz = sbt(p2, "kz", [128, 4, 2, SEQ], BF16)
            hm = sbt(p2, "hm", [128, 4], F32)
            vB1 = sbt(p2, "vB", [128, 16, 4, 128], BF16)
            vB = [vB1, vB1, vB1]
            acc = sbt(p2, "acc", [128, 4, SEQ], F32)
            mk = sbt(p2, "mk", [128, 3, 512], BF16)
            identb = sbt(p2, "identb", [128, 128], BF16)
            sk = sbt(p2, "sk", [128, 16], F32)
            ske = sbt(p2, "ske", [128, 16], F32)
            zer = sbt(p2, "zer", [128, 128], F32)
            sink512 = sbt(p2, "sink512", [128, 4, 512], F32)
            pS = [[pst(p2, "pS%d_%d" % (i, kb), [128, 512]) for kb in range(2)] for i in range(2)]
            pO = [pst(p2, "pO%d" % i, [128, 512]) for i in range(2)]
            P = [[sbt(p2, "P%d_%d" % (i, kb), [128, 512], BF16) for kb in range(2)] for i in range(2)]
            tl = [sbt(p2, "tl%d" % i, [128, 512], F32) for i in range(2)]
            rl = [sbt(p2, "rl%d" % i, [64, 512], F32) for i in range(2)]
            oA = [sbt(p2, "oA%d" % i, [64, 4, 512], BF16) for i in range(2)]
            rlB = sbt(p2, "rlB", [64, 4, 128], F32)
            oB = sbt(p2, "oB", [64, 4, 512], BF16)

            dma(gq, lambda: nc.gpsimd.dma_start(out=mk.ap[:, :, :], in_=masks_in[:, :, :]), writes=[mk])
            dma(gq, lambda: nc.gpsimd.dma_start(out=identb.ap[:, :], in_=ident_in[:, :]), writes=[identb])
            dma(sq, lambda: nc.sync.dma_start(out=hm.ap[:, :], in_=hm_in[:, :]), writes=[hm])
            dma(sq, lambda: nc.sync.dma_start(out=sk.ap[:, :], in_=sinks[0:1, :].partition_broadcast(128)), writes=[sk])
            op(act, lambda: nc.scalar.activation(out=ske.ap[:, :], in_=sk.ap[:, :], func=AF.Exp), reads=[sk], writes=[ske])
            op(pool, lambda: nc.gpsimd.memset(zer.ap[:, :], 0.0), writes=[zer])
            for j in range(4):
                for t in range(4):
                    h = 4 * j + t
                    op(dve, lambda j=j, t=t, h=h: nc.vector.tensor_scalar(out=sink512.ap[:, j, t * 128:(t + 1) * 128], in0=zer.ap[:, :],
                                                                          scalar1=ske.ap[:, h:h + 1], scalar2=None, op0=ALU.add),
                       reads=[zer, ske], writes=[sink512])
            MK_CUR, MK_PA, MK_PB = 0, 1, 2
            nu = 0
            for s in range(NSEQ):
                s0 = s * SEQ
                for t_ in range(10):
                    dma(sq, lambda t_=t_: nc.sync.dma_start(out=qk.ap[:, t_, :], in_=qk_scr[t_, :, s0:s0 + SEQ]), reads=[QK], writes=[qk])
                for part in range(2):
                    for hh_ in range(4):
                        op(dve, lambda part=part, hh_=hh_: nc.vector.tensor_scalar(out=kz.ap[:, hh_, part, :], in0=qk.ap[:, T_AK + part, :], scalar1=hm.ap[:, hh_:hh_ + 1], scalar2=None, op0=ALU.mult),
                           reads=[qk, hm], writes=[kz])
                vs = v_scr[s0:s0 + SEQ, :, :]
                dma(sq, lambda: nc.sync.dma_start(out=vA.ap[:, :, :, :], in_=vs[:, 0:4, :].rearrange("(b p) h d -> p b h d", p=128)), reads=[VS], writes=[vA])

                def unit(qsl, ksl, ksl_prev, vfn, mprev, is_a, j):
                    nonlocal nu
                    i = nu % 2
                    nu += 1
                    kbs = ([(0, ksl_prev, mprev)] if ksl_prev is not None else []) + [(1, ksl, MK_CUR)]
                    for (kb, ks, mkind) in kbs:
                        S = pS[i][kb]
                        fns = []
                        if is_a:
                            fns.append(lambda S=S, mkind=mkind: nc.tensor.matmul(S.ap[:, :], lhsT=identb.ap[:, :], rhs=mk.ap[:, mkind, :], start=True, stop=False))
                            for part in range(2):
                                fns.append(lambda part=part, ks=ks, S=S: nc.tensor.matmul(
                                    S.ap[:, :], lhsT=kz.ap[:, j, part, ks],
                                    rhs=qk.ap[:, T_AQ + part:T_AQ + 8:2, qsl], start=False, stop=(part == 1)))
                        else:
                            g = j[0]
                            for hh in range(4):
                                fns.append(lambda S=S, mkind=mkind, hh=hh: nc.tensor.matmul(S.ap[:, hh * 128:(hh + 1) * 128], lhsT=identb.ap[:, :], rhs=mk.ap[:, mkind, 0:128],
                                                                                            start=True, stop=False))
                                for part in range(2):
                                    fns.append(lambda part=part, hh=hh, ks=ks, S=S, g=g: nc.tensor.matmul(
                                        S.ap[:, hh * 128:(hh + 1) * 128], lhsT=kz.ap[:, hh, part, ks],
                                        rhs=qk.ap[:, T_BQ + 2 * g + part, qsl], start=False, stop=(part == 1)))
                        mm(pe, fns, reads=[qk, kz, mk, identb], writes=[S])
                        Pt = P[i][kb]
                        op(act, lambda S=S, Pt=Pt: nc.scalar.activation(out=Pt.ap[:, :], in_=S.ap[:, :], func=AF.Exp, scale=0.125), reads=[S], writes=[Pt])
                    O = pO[i]
                    fns = []
                    if is_a:
                        for n_, (kb, ks, mkind) in enumerate(kbs):
                            fns.append(lambda kb=kb, n_=n_: nc.tensor.matmul(O.ap[:, :], lhsT=vfn(kb, 0), rhs=P[i][kb].ap[:, :],
                                                                             start=(n_ == 0), stop=(n_ == len(kbs) - 1)))
                    else:
                        for hh in range(4):
                            for n_, (kb, ks, mkind) in enumerate(kbs):
                                fns.append(lambda kb=kb, n_=n_, hh=hh: nc.tensor.matmul(O.ap[:, hh * 128:(hh + 1) * 128], lhsT=vfn(kb, hh),
                                                                                        rhs=P[i][kb].ap[:, hh * 128:(hh + 1) * 128],
                                                                                        start=(n_ == 0), stop=(n_ == len(kbs) - 1)))
                    mm(pe, fns, reads=[P[i][kb] for (kb, _, _) in kbs] + [vA if is_a else vB[j[0]]], writes=[O])
                    return O, i

                for j in [int(c_) for c_ in os.environ.get("P2_A_J", "0123")]:
                    for bg in range(4):
                        ob = oA[(j * 4 + bg) % 2]
                        for bb in range(4):
                            b = bg * 4 + bb
                            qsl = slice(b * 128, (b + 1) * 128)
                            ksl = qsl
                            kprev = slice((b - 1) * 128, b * 128) if b > 0 else None
                            O, i = unit(qsl, ksl, kprev, lambda kb, hh, b=b, j=j: vA.ap[:, b - 1 + kb, j, :], MK_PA, True, j)
                            op(dve, lambda O=O, i=i, j=j: nc.vector.tensor_tensor(out=tl[i].ap[64:128, :], in0=O.ap[64:128, :], in1=sink512.ap[64:128, j, :], op=ALU.add),
                               reads=[O, sink512], writes=[tl[i]])
                            op(act, lambda i=i: nc.scalar.activation(out=tl[i].ap[64:128, :], in_=tl[i].ap[64:128, :], func=AF.Ln), reads=[tl[i]], writes=[tl[i]])
                            op(act, lambda i=i: nc.scalar.activation(out=rl[i].ap[:, :], in_=tl[i].ap[64:128, :], func=AF.Exp, scale=-1.0), reads=[tl[i]], writes=[rl[i]])
                            op(dve, lambda O=O, i=i, ob=ob, bb=bb: nc.vector.tensor_tensor(out=ob.ap[:, :, bb * 128:(bb + 1) * 128],
                                                                                          in0=O.ap[0:64, :].rearrange("p (t n) -> p t n", n=128),
                                                                                          in1=rl[i].ap[:, :].rearrange("p (t n) -> p t n", n=128), op=ALU.mult),
                               reads=[O, rl[i]], writes=[ob])
                        dst = oT_scr[256 * j:256 * j + 256, s0 + bg * 512:s0 + (bg + 1) * 512].rearrange("(t d) n -> d t n", d=64)
                        dma(sq, lambda dst=dst, ob=ob: nc.sync.dma_start(out=dst, in_=ob.ap[:, :, :]), reads=[ob], writes=[OT])

                for t_ in range(12):
                    dma(sq, lambda t_=t_: nc.sync.dma_start(out=qk.ap[:, t_, :], in_=qk_scr[10 + t_, :, s0:s0 + SEQ]), reads=[QK], writes=[qk])
                for g, (win, dil) in enumerate(B_GROUPS):
                    if str(g) not in os.environ.get("P2_B_G", "012"):
                        continue
                    nblk = 16 // dil
                    for part in range(2):
                        for hh_ in range(4):
                            op(dve, lambda part=part, g=g, hh_=hh_: nc.vector.tensor_scalar(out=kz.ap[:, hh_, part, :], in0=qk.ap[:, T_BK + 2 * g + part, :], scalar1=hm.ap[:, hh_:hh_ + 1], scalar2=None, op0=ALU.mult),
                               reads=[qk, hm], writes=[kz])
                    for r in range(dil):
                        src = vs[:, 4 + 4 * g:8 + 4 * g, :].rearrange("(cb p r) h d -> r p cb h d", p=128, r=dil)[r]
                        dma(sq, lambda g=g, r=r, nblk=nblk, src=src: nc.sync.dma_start(out=vB[g].ap[:, r * nblk:(r + 1) * nblk, :, :], in_=src),
                            reads=[VS], writes=[vB[g]])
                    for r in range(dil):
                        for cb in range(nblk):
                            base = r + dil * 128 * cb
                            qsl = slice(base, base + dil * 127 + 1, dil)
                            kprev = slice(base - dil * 128, base - dil * 128 + dil * 127 + 1, dil) if cb > 0 else None
                            u = r * nblk + cb
                            O, i = unit(qsl, qsl, kprev, lambda kb, hh, g=g, u=u: vB[g].ap[:, u - 1 + kb, hh, :], MK_PB, False, (g,))
                            dst = acc.ap[:, :, qsl]
                            src = O.ap[:, :].rearrange("p (h n) -> p h n", n=128)
                            if g == 0:
                                op(dve, lambda dst=dst, src=src: nc.vector.tensor_copy(out=dst, in_=src), reads=[O], writes=[acc])
                            else:
                                op(dve, lambda dst=dst, src=src: nc.vector.tensor_tensor(out=dst, in0=src, in1=dst, op=ALU.add), reads=[O, acc], writes=[acc])
                for bg in range(4):
                    for bb in range(4):
                        csl = slice(bg * 512 + bb * 128, bg * 512 + (bb + 1) * 128)
                        op(act, lambda csl=csl: nc.scalar.activation(out=rlB.ap[:, :, :], in_=acc.ap[64:128, :, csl], func=AF.Ln), reads=[acc], writes=[rlB])
                        op(act, lambda: nc.scalar.activation(out=rlB.ap[:, :, :], in_=rlB.ap[:, :, :], func=AF.Exp, scale=-1.0), reads=[rlB], writes=[rlB])
                        op(dve, lambda csl=csl, bb=bb: nc.vector.tensor_tensor(out=oB.ap[:, :, bb * 128:(bb + 1) * 128], in0=acc.ap[0:64, :, csl], in1=rlB.ap[:, :, :], op=ALU.mult),
                           reads=[acc, rlB], writes=[oB])
                    dst = oT_scr[1024:1280, s0 + bg * 512:s0 + (bg + 1) * 512].rearrange("(t d) n -> d t n", d=64)
                    dma(sq, lambda dst=dst: nc.sync.dma_start(out=dst, in_=oB.ap[:, :, :]), reads=[oB], writes=[OT])
            barrier()
        if stop_after <= 2:
            return nc

        def layer_norm(st_pool, hp, g_t, b_t, outt, tag, scr):
            stats, mv, rstd = scr["stats"], scr["mv"], scr["rstd"]
            for hf in range(2):
                op(dve, lambda hf=hf: nc.vector.bn_stats(out=stats.ap[:, hf, :], in_=hp.ap[:, hf * 512:(hf + 1) * 512]), reads=[hp], writes=[stats])
            op(dve, lambda: nc.vector.bn_aggr(out=mv.ap[:, :], in_=stats.ap[:, :, :].rearrange("p a b -> p (a b)")), reads=[stats], writes=[mv])
            op(dve, lambda: nc.vector.tensor_scalar(out=rstd.ap[:, :], in0=mv.ap[:, 1:2], scalar1=EPS, scalar2=None, op0=ALU.add), reads=[mv], writes=[rstd])
            op(act, lambda: nc.scalar.activation(out=rstd.ap[:, :], in_=rstd.ap[:, :], func=AF.Sqrt), reads=[rstd], writes=[rstd])
            op(dve, lambda: nc.vector.reciprocal(out=rstd.ap[:, :], in_=rstd.ap[:, :]), reads=[rstd], writes=[rstd])
            op(dve, lambda: nc.vector.tensor_scalar(out=outt.ap[:, :], in0=hp.ap[:, :], scalar1=mv.ap[:, 0:1], scalar2=rstd.ap[:, 0:1],
                                                    op0=ALU.subtract, op1=ALU.mult), reads=[hp, mv, rstd], writes=[outt])
            op(dve, lambda: nc.vector.tensor_tensor(out=outt.ap[:, :], in0=outt.ap[:, :], in1=g_t.ap[:, :], op=ALU.mult), reads=[outt, g_t], writes=[outt])
            op(dve, lambda: nc.vector.tensor_tensor(out=outt.ap[:, :], in0=outt.ap[:, :], in1=b_t.ap[:, :], op=ALU.add), reads=[outt, b_t], writes=[outt])

        with ExitStack() as p3:
            wg = sbt(p3, "wg", [128, 8, 2048], BF16)
            wa = sbt(p3, "wa", [128, 8, D], BF16)
            wb = sbt(p3, "wb", [128, 2, D], BF16)
            wo = sbt(p3, "wo", [128, 8, D], BF16)
            wr = sbt(p3, "wr", [128, 8, NEXP], F32)
            brb = sbt(p3, "brb", [128, NEXP], F32)
            g1 = sbt(p3, "g1", [128, D], F32); b1 = sbt(p3, "b1", [128, D], F32)
            xT3 = [sbt(p3, "xT3_%d" % i, [128, 8, 512], BF16) for i in range(2)]
            oT = [sbt(p3, "oT%d" % i, [128, 10, 512], BF16) for i in range(2)]
            mT = [sbt(p3, "mT%d" % i, [128, 8, 512], BF16) for i in range(2)]
            pG = [pst(p3, "pG%d" % i, [128, 512]) for i in range(2)]
            pY = [pst(p3, "pY%d" % i, [128, 512]) for i in range(2)]
            pOut = [pst(p3, "pOut%d" % i, [128, 512]) for i in range(2)]
            pTr = pG
            sg = [sbt(p3, "sg%d" % i, [128, 512], F32) for i in range(2)]
            m1 = sbt(p3, "m1", [128, 512], F32); m2 = sbt(p3, "m2", [128, 512], F32)
            xres = sbt(p3, "xres", [128, D], F32)
            hp = sbt(p3, "hp", [128, D], F32)
            h1s = [sbt(p3, "h1_%d" % i, [128, D], F32) for i in range(4)]
            h1bs = [sbt(p3, "h1b%d" % i, [128, D], BF16) for i in range(4)]
            h1T = [sbt(p3, "h1T%d" % i, [128, 8, 128], F32) for i in range(2)]
            lnscr = {"stats": sbt(p3, "stats", [128, 2, 6], F32), "mv": sbt(p3, "mv", [128, 2], F32), "rstd": sbt(p3, "rstd", [128, 1], F32)}
            pL = pst(p3, "pL", [128, 512])
            pLt = [pL, pL, pL, pL]
            lgs = [sbt(p3, "lg%d" % i, [128, NEXP], F32) for i in range(4)]
            m8s = [sbt(p3, "m8_%d" % i, [128, 8], F32) for i in range(4)]
            nm0 = sbt(p3, "nm0", [128, 1], F32)
            masks_ = [sbt(p3, "mask%d" % i, [128, NEXP], F32) for i in range(4)]
            maskbs = [sbt(p3, "maskb%d" % i, [128, NEXP], BF16) for i in range(4)]
            ee = sbt(p3, "ee", [128, NEXP], F32)
            ssum = sbt(p3, "ssum", [128, 1], F32)
            rs = sbt(p3, "rs", [128, 1], F32)
            W = sbt(p3, "W", [128, NEXP], F32)
            basec = sbt(p3, "basec", [128, NEXP], F32)
            posf = sbt(p3, "posf", [128, NEXP], F32)
            ovf = sbt(p3, "ovf", [128, NEXP], F32)
            oh = sbt(p3, "oh", [128, NEXP], F32)
            junk = sbt(p3, "junk", [128, NEXP], F32)
            slotf = sbt(p3, "slotf", [128, 4], F32)

            wgv = w_inp.rearrange("(c p) n -> p c n", p=128)
            dma(gq, lambda: nc.gpsimd.dma_start(out=wg.ap[:, :, 0:1024], in_=wgv[:, :, 3840:4864]), writes=[wg])
            dma(gq, lambda: nc.gpsimd.dma_start(out=wg.ap[:, :, 1024:2048], in_=wgv[:, :, 4864:5888]), writes=[wg])
            dma(gq, lambda: nc.gpsimd.dma_start(out=wa.ap[:, :, :], in_=w_a.rearrange("(c p) n -> p c n", p=128)), writes=[wa])
            dma(gq, lambda: nc.gpsimd.dma_start(out=wb.ap[:, :, :], in_=w_b.rearrange("(c p) n -> p c n", p=128)), writes=[wb])
            dma(gq, lambda: nc.gpsimd.dma_start(out=wo.ap[:, :, :], in_=w_o.rearrange("(c p) n -> p c n", p=128)), writes=[wo])
            dma(sq, lambda: nc.sync.dma_start(out=wr.ap[:, :, :], in_=w_r.rearrange("(c p) n -> p c n", p=128)), writes=[wr])
            dma(sq, lambda: nc.sync.dma_start(out=brb.ap[:, :], in_=b_r[0:1, :].partition_broadcast(128)), writes=[brb])
            dma(sq, lambda: nc.sync.dma_start(out=g1.ap[:, :], in_=ln1_g[0:1, :].partition_broadcast(128)), writes=[g1])
            dma(sq, lambda: nc.sync.dma_start(out=b1.ap[:, :], in_=ln1_b[0:1, :].partition_broadcast(128)), writes=[b1])
            op(pool, lambda: nc.gpsimd.memset(basec.ap[:, :], 0.0), writes=[basec])

            ng = [0]

            def stage_load(ch):
                tok0 = ch * 512
                xt_, ot_ = xT3[ch % 2], oT[ch % 2]
                for c in range(8):
                    dma(sq, lambda c=c: nc.sync.dma_start_transpose(out=xt_.ap[:, c, :], in_=xb[tok0:tok0 + 512, c * 128:(c + 1) * 128]),
                        reads=[XBs[ch // 2]], writes=[xt_])
                dma(sq, lambda: nc.sync.dma_start(out=ot_.ap[:, :, :], in_=oT_scr[:, tok0:tok0 + 512].rearrange("(c p) n -> p c n", p=128)),
                    reads=[OT], writes=[ot_])

            def stage_gate(ch):
                xt_, ot_, mt_ = xT3[ch % 2], oT[ch % 2], mT[ch % 2]
                for m in range(8):
                    for br in range(2):
                        G = pG[ng[0] % 2]; Y = pY[ng[0] % 2]; S_ = sg[ng[0] % 2]
                        ng[0] += 1
                        col = br * 1024 + m * 128
                        mm(pe, [lambda c=c: nc.tensor.matmul(G.ap[:, :], lhsT=wg.ap[:, c, col:col + 128], rhs=xt_.ap[:, c, :],
                                                             start=(c == 0), stop=(c == 7)) for c in range(8)], reads=[wg, xt_], writes=[G])
                        if br == 0:
                            mm(pe, [lambda c=c: nc.tensor.matmul(Y.ap[:, :], lhsT=wa.ap[:, c, m * 128:(m + 1) * 128], rhs=ot_.ap[:, c, :],
                                                                 start=(c == 0), stop=(c == 7)) for c in range(8)], reads=[wa, ot_], writes=[Y])
                        else:
                            mm(pe, [lambda c=c: nc.tensor.matmul(Y.ap[:, :], lhsT=wb.ap[:, c, m * 128:(m + 1) * 128], rhs=ot_.ap[:, 8 + c, :],
                                                                 start=(c == 0), stop=(c == 1)) for c in range(2)], reads=[wb, ot_], writes=[Y])
                        op(act, lambda: nc.scalar.activation(out=S_.ap[:, :], in_=G.ap[:, :], func=AF.Sigmoid), reads=[G], writes=[S_])
                        mo = m1 if br == 0 else m2
                        op(dve, lambda: nc.vector.tensor_tensor(out=mo.ap[:, :], in0=Y.ap[:, :], in1=S_.ap[:, :], op=ALU.mult),
                           reads=[Y, S_], writes=[mo])
                    op(pool, lambda: nc.gpsimd.tensor_tensor(out=mt_.ap[:, m, :], in0=m1.ap[:, :], in1=m2.ap[:, :], op=ALU.add), reads=[m1, m2], writes=[mt_])

            def stage_out(ch):
                tok0 = ch * 512
                mt_ = mT[ch % 2]
                for tt in range(4):
                    r0 = tok0 + tt * 128
                    h1_, h1b_ = h1s[tt], h1bs[tt]
                    dma(sq, lambda: nc.sync.dma_start(out=xres.ap[:, :], in_=x[r0:r0 + 128, :]), writes=[xres])
                    for hf in range(2):
                        mm(pe, [lambda m=m: nc.tensor.matmul(pOut[hf].ap[:, :], lhsT=mt_.ap[:, m, tt * 128:(tt + 1) * 128], rhs=wo.ap[:, m, hf * 512:(hf + 1) * 512],
                                                             start=(m == 0), stop=(m == 7)) for m in range(8)], reads=[mt_, wo], writes=[pOut[hf]])
                        op(dve, lambda: nc.vector.scalar_tensor_tensor(out=hp.ap[:, hf * 512:(hf + 1) * 512], in0=xres.ap[:, hf * 512:(hf + 1) * 512], scalar=ALPHA,
                                                                       in1=pOut[hf].ap[:, :], op0=ALU.mult, op1=ALU.add), reads=[xres, pOut[hf]], writes=[hp])
                    layer_norm(p3, hp, g1, b1, h1_, "ln1", lnscr)
                    dma(sq, lambda: nc.sync.dma_start(out=h1_scr[r0:r0 + 128, :], in_=h1_.ap[:, :]), reads=[h1_], writes=[H1])
                    op(act, lambda: nc.scalar.copy(out=h1b_.ap[:, :], in_=h1_.ap[:, :]), reads=[h1_], writes=[h1b_])

            def stage_route(ch):
                for tt in range(4):
                    h1_ = h1s[tt]
                    lg, m8, mask, maskb = lgs[tt], m8s[tt], masks_[tt], maskbs[tt]
                    hT_ = h1T[tt % 2]
                    for hf in range(2):
                        mm(pe, [lambda c=c: nc.tensor.transpose(pTr[hf].ap[:, (c % 4) * 128:(c % 4 + 1) * 128], h1_.ap[:, c * 128:(c + 1) * 128], ident.ap[:, :])
                                for c in range(hf * 4, hf * 4 + 4)], reads=[h1_, ident], writes=[pTr[hf]])
                        op(act, lambda: nc.scalar.copy(out=hT_.ap[:, hf * 4:(hf + 1) * 4, :], in_=pTr[hf].ap[:, :].rearrange("p (c n) -> p c n", n=128)),
                           reads=[pTr[hf]], writes=[hT_])
                    L0 = tt * 128
                    mm(pe, [lambda c=c: nc.tensor.matmul(pL.ap[:, L0:L0 + NEXP], lhsT=hT_.ap[:, c, :], rhs=wr.ap[:, c, :], start=(c == 0), stop=(c == 7)) for c in range(8)],
                       reads=[hT_, wr], writes=[pLt[tt]])
                    op(dve, lambda: nc.vector.tensor_tensor(out=lg.ap[:, :], in0=pL.ap[:, L0:L0 + NEXP], in1=brb.ap[:, :], op=ALU.add), reads=[pLt[tt], brb], writes=[lg])
                    op(dve, lambda: nc.vector.max(out=m8.ap[:, :], in_=lg.ap[:, :]), reads=[lg], writes=[m8])
                    op(dve, lambda: nc.vector.tensor_scalar(out=mask.ap[:, :], in0=lg.ap[:, :], scalar1=m8.ap[:, 3:4], scalar2=None, op0=ALU.is_ge), reads=[lg, m8], writes=[mask])
                    op(dve, lambda: nc.vector.tensor_copy(out=maskb.ap[:, :], in_=mask.ap[:, :]), reads=[mask], writes=[maskb])
                for tt in range(4):
                    ti = ch * 4 + tt
                    h1b_ = h1bs[tt]
                    lg, m8, mask, maskb = lgs[tt], m8s[tt], masks_[tt], maskbs[tt]
                    L0 = tt * 128
                    op(dve, lambda: nc.vector.tensor_scalar(out=nm0.ap[:, :], in0=m8.ap[:, 0:1], scalar1=-1.0, scalar2=None, op0=ALU.mult), reads=[m8], writes=[nm0])
                    op(act, lambda: nc.scalar.activation(out=ee.ap[:, :], in_=lg.ap[:, :], func=AF.Exp, bias=nm0.ap[:, 0:1], scale=1.0), reads=[lg, nm0], writes=[ee])
                    op(dve, lambda: nc.vector.tensor_tensor(out=ee.ap[:, :], in0=ee.ap[:, :], in1=mask.ap[:, :], op=ALU.mult), reads=[ee, mask], writes=[ee])
                    op(dve, lambda: nc.vector.reduce_sum(out=ssum.ap[:, :], in_=ee.ap[:, :], axis=mybir.AxisListType.X), reads=[ee], writes=[ssum])
                    op(dve, lambda: nc.vector.reciprocal(out=rs.ap[:, :], in_=ssum.ap[:, :]), reads=[ssum], writes=[rs])
                    op(dve, lambda: nc.vector.tensor_scalar(out=W.ap[:, :], in0=ee.ap[:, :], scalar1=rs.ap[:, 0:1], scalar2=None, op0=ALU.mult), reads=[ee, rs], writes=[W])
                    mm(pe, [lambda: nc.tensor.matmul(pL.ap[:, L0 + 32:L0 + 64], lhsT=tri.ap[:, :], rhs=maskb.ap[:, :], start=True, stop=True),
                            lambda: nc.tensor.matmul(pL.ap[:, L0 + 64:L0 + 96], lhsT=onesb.ap[:, :], rhs=maskb.ap[:, :], start=True, stop=True)],
                       reads=[tri, onesb, maskb], writes=[pLt[tt]])
                    op(dve, lambda: nc.vector.tensor_tensor(out=posf.ap[:, :], in0=pL.ap[:, L0 + 32:L0 + 64], in1=basec.ap[:, :], op=ALU.add), reads=[pLt[tt], basec], writes=[posf])
                    op(dve, lambda: nc.vector.tensor_tensor(out=basec.ap[:, :], in0=pL.ap[:, L0 + 64:L0 + 96], in1=basec.ap[:, :], op=ALU.add), reads=[pLt[tt], basec], writes=[basec])
                    op(dve, lambda: nc.vector.tensor_scalar(out=ovf.ap[:, :], in0=posf.ap[:, :], scalar1=float(C), scalar2=None, op0=ALU.is_lt), reads=[posf], writes=[ovf])
                    op(dve, lambda: nc.vector.tensor_tensor(out=W.ap[:, :], in0=W.ap[:, :], in1=ovf.ap[:, :], op=ALU.mult), reads=[W, ovf], writes=[W])
                    op(dve, lambda: nc.vector.tensor_scalar(out=ovf.ap[:, :], in0=ovf.ap[:, :], scalar1=-1.0, scalar2=-4.0e6, op0=ALU.add, op1=ALU.mult), reads=[ovf], writes=[ovf])
                    op(dve, lambda: nc.vector.tensor_tensor(out=posf.ap[:, :], in0=posf.ap[:, :], in1=ec.ap[:, :], op=ALU.add), reads=[posf, ec], writes=[posf])
                    op(dve, lambda: nc.vector.tensor_tensor(out=posf.ap[:, :], in0=posf.ap[:, :], in1=ovf.ap[:, :], op=ALU.add), reads=[posf, ovf], writes=[posf])
                    for k in range(4):
                        op(dve, lambda: nc.vector.tensor_scalar(out=oh.ap[:, :], in0=lg.ap[:, :], scalar1=m8.ap[:, k:k + 1], scalar2=None, op0=ALU.is_equal), reads=[lg, m8], writes=[oh])
                        op(dve, lambda: nc.vector.tensor_tensor(out=junk.ap[:, :], in0=oh.ap[:, :], in1=posf.ap[:, :], op=ALU.mult), reads=[oh, posf], writes=[junk])
                        op(dve, lambda: nc.vector.reduce_sum(out=slotf.ap[:, k:k + 1], in_=junk.ap[:, :], axis=mybir.AxisListType.X), reads=[junk], writes=[slotf])
                        op(dve, lambda: nc.vector.tensor_tensor(out=junk.ap[:, :], in0=oh.ap[:, :], in1=W.ap[:, :], op=ALU.mult), reads=[oh, W], writes=[junk])
                        op(dve, lambda: nc.vector.reduce_sum(out=wts_all.ap[:, ti, k:k + 1], in_=junk.ap[:, :], axis=mybir.AxisListType.X), reads=[junk], writes=[wts_all])
                    op(dve, lambda: nc.vector.tensor_copy(out=slots_all.ap[:, ti, :], in_=slotf.ap[:, :]), reads=[slotf], writes=[slots_all])
                    for k in range(4):
                        dma(gq, lambda: nc.gpsimd.indirect_dma_start(
                            out=xg_scr[:, :], out_offset=bass.IndirectOffsetOnAxis(ap=slots_all.ap[:, ti, k:k + 1], axis=0),
                            in_=h1b_.ap[:, :], in_offset=None, bounds_check=bc_reg, oob_is_err=False),
                            reads=[h1b_, slots_all], writes=[XG])

            stage_load(0)
            stage_gate(0)
            for ch in range(NCH):
                if ch + 1 < NCH:
                    stage_load(ch + 1)
                if ch > 0:
                    stage_route(ch - 1)
                stage_out(ch)
                if ch + 1 < NCH:
                    stage_gate(ch + 1)
            stage_route(NCH - 1)
            barrier()
        if stop_after <= 3:
            return nc

        with ExitStack() as p4:
            wgu = [sbt(p4, "wgu%d" % i, [128, 8, 2 * D], BF16) for i in range(2)]
            wd = [sbt(p4, "wd%d" % i, [128, 8, D], BF16) for i in range(2)]
            bgu = [sbt(p4, "bgu%d" % i, [128, 16], F32) for i in range(2)]
            bdr = [sbt(p4, "bdr%d" % i, [1, D], BF16) for i in range(2)]
            xg = [sbt(p4, "xg%d" % i, [128, 8, 512], BF16) for i in range(2)]
            hT = [sbt(p4, "hT%d" % i, [128, 8, 512], BF16) for i in range(2)]
            pGg = [pst(p4, "pGg%d" % i, [128, 512]) for i in range(2)]
            pUu = [pst(p4, "pUu%d" % i, [128, 512]) for i in range(2)]
            pYy = [pst(p4, "pYy%d" % i, [128, 512]) for i in range(4)]
            gs_ = [sbt(p4, "gsb%d" % i, [128, 512], F32) for i in range(2)]
            ss_ = [sbt(p4, "ssb%d" % i, [128, 512], F32) for i in range(2)]
            us_ = [sbt(p4, "usb%d" % i, [128, 512], F32) for i in range(2)]
            ysb = [sbt(p4, "ysb%d" % i, [128, D], F32) for i in range(2)]
            batches = []
            s_ = 0
            BS = 384 if C % 384 == 0 else 512
            while s_ < C:
                batches.append((s_, min(BS, C - s_)))
                s_ += BS
            nm = 0
            ny = 0
            items = [(e, sb0, S) for e in range(NEXP) for (sb0, S) in batches]

            def load_w(e):
                i2 = e % 2
                for c0 in range(0, 2048, 1024):
                    dma(gq, lambda c0=c0: nc.gpsimd.dma_start(out=wgu[i2].ap[:, :, c0:c0 + 1024], in_=w_gu[e].rearrange("(c p) n -> p c n", p=128)[:, :, c0:c0 + 1024]),
                        writes=[wgu[i2]])
                dma(gq, lambda: nc.gpsimd.dma_start(out=wd[i2].ap[:, :, :], in_=w_d[e].rearrange("(c p) n -> p c n", p=128)), writes=[wd[i2]])
                dma(gq, lambda: nc.gpsimd.dma_start(out=bgu[i2].ap[:, :], in_=b_gu[e].rearrange("(m p) -> p m", p=128), allow_slow_non_contiguous=True), writes=[bgu[i2]])
                dma(gq, lambda: nc.gpsimd.dma_start(out=bdr[i2].ap[:, :], in_=b_d[e:e + 1, :]), writes=[bdr[i2]])

            def load_xg(idx):
                e, sb0, S = items[idx]
                xgt = xg[idx % 2]
                r0 = e * C + sb0
                for c in range(8):
                    dma(sq, lambda c=c: nc.sync.dma_start_transpose(out=xgt.ap[:, c, 0:S], in_=xg_scr[r0:r0 + S, c * 128:(c + 1) * 128]),
                        reads=[XG], writes=[xgt])

            load_w(0)
            load_xg(0)
            for idx, (e, sb0, S) in enumerate(items):
                i2 = e % 2
                if sb0 == 0:
                    op(dve, lambda: nc.vector.tensor_scalar(out=bgu[i2].ap[:, 8:16], in0=bgu[i2].ap[:, 8:16], scalar1=1.0, scalar2=None, op0=ALU.add), reads=[bgu[i2]], writes=[bgu[i2]])
                    if e + 1 < NEXP:
                        load_w(e + 1)
                if idx + 1 < len(items):
                    load_xg(idx + 1)
                xgt = xg[idx % 2]; ht = hT[idx % 2]
                r0 = e * C + sb0
                for m in range(8):
                    Gp = pGg[nm % 2]; Up = pUu[nm % 2]; gsb = gs_[nm % 2]; ssb = ss_[nm % 2]; usb = us_[nm % 2]
                    nm += 1
                    mm(pe, [lambda c=c: nc.tensor.matmul(Gp.ap[:, 0:S], lhsT=wgu[i2].ap[:, c, m * 128:(m + 1) * 128], rhs=xgt.ap[:, c, 0:S],
                                                         start=(c == 0), stop=(c == 7)) for c in range(8)], reads=[wgu[i2], xgt], writes=[Gp])
                    mm(pe, [lambda c=c: nc.tensor.matmul(Up.ap[:, 0:S], lhsT=wgu[i2].ap[:, c, D + m * 128:D + (m + 1) * 128], rhs=xgt.ap[:, c, 0:S],
                                                         start=(c == 0), stop=(c == 7)) for c in range(8)], reads=[wgu[i2], xgt], writes=[Up])
                    op(dve, lambda: nc.vector.tensor_scalar(out=gsb.ap[:, 0:S], in0=Gp.ap[:, 0:S], scalar1=bgu[i2].ap[:, m:m + 1], scalar2=7.0,
                                                            op0=ALU.add, op1=ALU.min), reads=[Gp, bgu[i2]], writes=[gsb])
                    op(act, lambda: nc.scalar.activation(out=ssb.ap[:, 0:S], in_=gsb.ap[:, 0:S], func=AF.Sigmoid, scale=1.702), reads=[gsb], writes=[ssb])
                    op(dve, lambda: nc.vector.tensor_scalar(out=usb.ap[:, 0:S], in0=Up.ap[:, 0:S], scalar1=bgu[i2].ap[:, 8 + m:9 + m], scalar2=8.0,
                                                            op0=ALU.add, op1=ALU.min), reads=[Up, bgu[i2]], writes=[usb])
                    op(pool, lambda: nc.gpsimd.tensor_tensor(out=gsb.ap[:, 0:S], in0=gsb.ap[:, 0:S], in1=ssb.ap[:, 0:S], op=ALU.mult),
                       reads=[gsb, ssb], writes=[gsb])
                    op(dve, lambda: nc.vector.scalar_tensor_tensor(out=ht.ap[:, m, 0:S], in0=usb.ap[:, 0:S], scalar=-6.0, in1=gsb.ap[:, 0:S],
                                                                   op0=ALU.max, op1=ALU.mult), reads=[gsb, usb], writes=[ht])
                for st in range(S // 128):
                    yb = ysb[ny % 2]
                    for hf in range(2):
                        Yp = pYy[(2 * ny + hf) % 4]
                        fns = [lambda m=m: nc.tensor.matmul(Yp.ap[:, :], lhsT=ht.ap[:, m, st * 128:(st + 1) * 128], rhs=wd[i2].ap[:, m, hf * 512:(hf + 1) * 512],
                                                            start=(m == 0), stop=False) for m in range(8)]
                        fns.append(lambda: nc.tensor.matmul(Yp.ap[:, :], lhsT=onesb.ap[0:1, :], rhs=bdr[i2].ap[0:1, hf * 512:(hf + 1) * 512], start=False, stop=True))
                        mm(pe, fns, reads=[ht, wd[i2], bdr[i2], onesb], writes=[Yp])
                        op(act, lambda: nc.scalar.copy(out=yb.ap[:, hf * 512:(hf + 1) * 512], in_=Yp.ap[:, :]), reads=[Yp], writes=[yb])
                    ny += 1
                    rr = r0 + st * 128
                    dma(sq, lambda: nc.sync.dma_start(out=y_scr[rr:rr + 128, :], in_=yb.ap[:, :]), reads=[yb], writes=[YS])
            barrier()
        if stop_after <= 4:
            return nc

        with ExitStack() as p5:
            g2 = sbt(p5, "g2", [128, D], F32); b2 = sbt(p5, "b2", [128, D], F32)
            yk = [[sbt(p5, "yk%d_%d" % (i, k), [128, D], F32) for k in range(4)] for i in range(2)]
            hres = [sbt(p5, "hres%d" % i, [128, D], F32) for i in range(2)]
            acc5 = [sbt(p5, "acc5_%d" % i, [128, D], F32) for i in range(2)]
            o5 = [sbt(p5, "o5_%d" % i, [128, D], F32) for i in range(2)]
            lnscr5 = [{"stats": sbt(p5, "stats5%d" % i, [128, 2, 6], F32), "mv": sbt(p5, "mv5%d" % i, [128, 2], F32), "rstd": sbt(p5, "rstd5%d" % i, [128, 1], F32)} for i in range(2)]
            dma(sq, lambda: nc.sync.dma_start(out=g2.ap[:, :], in_=ln2_g[0:1, :].partition_broadcast(128)), writes=[g2])
            dma(sq, lambda: nc.sync.dma_start(out=b2.ap[:, :], in_=ln2_b[0:1, :].partition_broadcast(128)), writes=[b2])
            for i in range(2):
                for k in range(4):
                    op(pool, lambda i=i, k=k: nc.gpsimd.memset(yk[i][k].ap[:, :], 0.0), writes=[yk[i][k]])
            for ti in range(NTT):
                i = ti % 2
                r0 = ti * 128
                dma(sq, lambda r0=r0, i=i: nc.sync.dma_start(out=hres[i].ap[:, :], in_=h1_scr[r0:r0 + 128, :]), reads=[H1], writes=[hres[i]])
                for k in range(4):
                    dma(gq, lambda k=k, ti=ti, i=i: nc.gpsimd.indirect_dma_start(
                        out=yk[i][k].ap[:, :], out_offset=None, in_=y_scr[:, :],
                        in_offset=bass.IndirectOffsetOnAxis(ap=slots_all.ap[:, ti, k:k + 1], axis=0), bounds_check=bc_reg, oob_is_err=False),
                        reads=[YS, slots_all], writes=[yk[i][k]])
                a5 = acc5[i]
                op(dve, lambda i=i, ti=ti, a5=a5: nc.vector.tensor_scalar(out=a5.ap[:, :], in0=yk[i][0].ap[:, :], scalar1=wts_all.ap[:, ti, 0:1], scalar2=None, op0=ALU.mult),
                   reads=[yk[i][0], wts_all], writes=[a5])
                for k in range(1, 4):
                    eng = dve
                    ee_ = nc.vector
                    op(eng, lambda i=i, ti=ti, k=k, a5=a5, ee_=ee_: ee_.scalar_tensor_tensor(out=a5.ap[:, :], in0=yk[i][k].ap[:, :], scalar=wts_all.ap[:, ti, k:k + 1], in1=a5.ap[:, :],
                                                                                          op0=ALU.mult, op1=ALU.add), reads=[yk[i][k], wts_all, a5], writes=[a5])
                op(dve, lambda i=i, a5=a5: nc.vector.scalar_tensor_tensor(out=a5.ap[:, :], in0=hres[i].ap[:, :], scalar=ALPHA, in1=a5.ap[:, :], op0=ALU.mult, op1=ALU.add),
                   reads=[hres[i], a5], writes=[a5])
                layer_norm(p5, a5, g2, b2, o5[i], "ln2", lnscr5[i])
                dma(sq, lambda r0=r0, i=i: nc.sync.dma_start(out=out[r0:r0 + 128, :], in_=o5[i].ap[:, :]), reads=[o5[i]])
            barrier()
    return nc


def _perm_cols():
    OFF_AQ, OFF_AK, OFF_AV, OFF_BQ, OFF_BK, OFF_BV, OFF_G = 0, 1024, 1280, 1536, 2304, 3072, 3840
    cols = []

    def tile(heads_off, part):
        for off in heads_off:
            cols.extend(range(off + 32 * part, off + 32 * part + 32))

    for t in range(4):
        for part in range(2):
            tile([OFF_AQ + (4 * j + t) * 64 for j in range(4)], part)
    for part in range(2):
        tile([OFF_AK + j * 64 for j in range(4)], part)
    for g in range(3):
        for part in range(2):
            tile([OFF_BQ + (4 * g + j) * 64 for j in range(4)], part)
    for g in range(3):
        for part in range(2):
            tile([OFF_BK + (4 * g + j) * 64 for j in range(4)], part)
    cols.extend(range(OFF_AV, OFF_AV + 256))
    cols.extend(range(OFF_BV, OFF_BV + 768))
    cols.extend(range(OFF_G, OFF_G + 2048))
    return np.asarray(cols, dtype=np.int64)


def _consts(C):
    p = np.arange(128)
    inv_freq = 10000.0 ** (-(np.arange(32, dtype=np.float64)) / 32.0)
    ang = np.arange(SEQ, dtype=np.float64)[None, :] * inv_freq[p % 32][:, None]
    cos_t = np.cos(ang).astype(np.float32)
    sin_t = np.sin(ang).astype(np.float32)
    k = np.arange(128)[:, None]
    q = np.arange(128)[None, :]
    m_cur = (k <= q)
    m_pa = (k >= q + 1)
    m_pb = (k >= q)
    masks = np.stack([np.tile(np.where(m, 0.0, -30000.0), (1, 4)) for m in (m_cur, m_pa, m_pb)], axis=1).astype(np.float32)
    tri = (np.arange(128)[:, None] < np.arange(128)[None, :]).astype(np.float32)
    ident = np.eye(128, dtype=np.float32)
    ec = np.tile((np.arange(NEXP) * C).astype(np.float32)[None, :], (128, 1))
    hm = (np.arange(128)[:, None] // 32 == np.arange(4)[None, :]).astype(np.float32)
    return dict(cos_t=cos_t, sin_t=sin_t, masks=masks, tri=tri, ident=ident, ec=ec, hm=hm)


def make_in_maps(inputs, ncores, NSEQ, C):
    x = np.asarray(inputs["x"], dtype=np.float32)
    perm = _perm_cols()
    w_inp = np.ascontiguousarray(np.asarray(inputs["w_in"])[0][:, perm])
    shared = dict(
        w_inp=w_inp,
        w_a=np.ascontiguousarray(inputs["w_branch_a"][0]), w_b=np.ascontiguousarray(inputs["w_branch_b"][0]),
        w_o=np.ascontiguousarray(inputs["w_out"][0]),
        sinks=np.ascontiguousarray(inputs["attn_sinks"]).reshape(1, 16),
        ln1_g=np.asarray(inputs["ln1_g"]).reshape(1, D), ln1_b=np.asarray(inputs["ln1_b"]).reshape(1, D),
        ln2_g=np.asarray(inputs["ln2_g"]).reshape(1, D), ln2_b=np.asarray(inputs["ln2_b"]).reshape(1, D),
        w_r=np.ascontiguousarray(inputs["w_router"][0]), b_r=np.asarray(inputs["b_router"]).reshape(1, NEXP),
        w_gu=np.ascontiguousarray(inputs["w_gate_up"][0]), b_gu=np.ascontiguousarray(inputs["b_gate_up"][0]),
        w_d=np.ascontiguousarray(inputs["w_down"][0]), b_d=np.ascontiguousarray(inputs["b_down"][0]),
    )
    shared = {k: np.asarray(v, dtype=np.float32) for k, v in shared.items()}
    shared.update(_consts(C))
    maps = []
    for c in range(ncores):
        m = dict(shared)
        m["x"] = np.ascontiguousarray(x[c * NSEQ:(c + 1) * NSEQ].reshape(NSEQ * SEQ, D))
        maps.append(m)
    return maps


def kernel(**inputs):
    ncores, NSEQ, C = 8, 4, 1152
    nc = build(NSEQ=NSEQ, C=C)
    in_maps = make_in_maps(inputs, ncores, NSEQ, C)
    res = run_bass_kernel_spmd(nc, in_maps, core_ids=list(range(ncores)))
    outs = [np.asarray(r["out"], dtype=np.float32).reshape(NSEQ, SEQ, D) for r in res.results]
    return np.concatenate(outs, axis=0)
```

```python
import os
import numpy as np
from contextlib import ExitStack
import concourse.bass as bass
import concourse.mybir as mybir
from concourse.bass_utils import run_bass_kernel_spmd

F32 = mybir.dt.float32
BF16 = mybir.dt.bfloat16
I32 = mybir.dt.int32
AF = mybir.ActivationFunctionType
ALU = mybir.AluOpType

D = 1024
SEQ = 2048
NEXP = 32
ALPHA = 2.0 ** 0.25
EPS = 1e-5
NQK = 22
T_AQ = 0
T_AK = 8
T_BQ = 0
T_BK = 6
B_GROUPS = ((128, 1), (512, 4), (2048, 16))


class Eng:
    def __init__(self, nc, eng, name, es):
        self.e = eng
        self.name = name
        self.sem = es.enter_context(nc.semaphore("s_" + name))
        self.count = 0
        self.seen = {}

    def wait(self, tok):
        if tok is None:
            return
        sem, v, key = tok
        if self.seen.get(key, 0) >= v:
            return
        self.seen[key] = v
        self.e.wait_ge(sem, v)

    def issue(self, ins):
        ins.then_inc(self.sem, 1)
        self.count += 1
        return (self.sem, self.count, self.name)

    def alltoks(self):
        return [(self.sem, self.count, self.name)] if self.count else []


class DmaQ:
    def __init__(self, nc, eng, name, es, nsems=8):
        self.e = eng
        self.name = name
        self.sems = [es.enter_context(nc.semaphore("d_%s%d" % (name, i))) for i in range(nsems)]
        self.counts = [0] * nsems
        self.n = 0
        self.seen = {}

    def wait(self, tok):
        if tok is None:
            return
        sem, v, key = tok
        if self.seen.get(key, 0) >= v:
            return
        self.seen[key] = v
        self.e.wait_ge(sem, v)

    def issue(self, fn):
        i = self.n % len(self.sems)
        self.n += 1
        key = "%s_%d" % (self.name, i)
        if self.counts[i] > 0:
            self.wait((self.sems[i], self.counts[i], key))
        ins = fn()
        ins.then_inc(self.sems[i], 16)
        self.counts[i] += 16
        return (self.sems[i], self.counts[i], key)

    def alltoks(self):
        return [(s, c, "%s_%d" % (self.name, i)) for i, (s, c) in enumerate(zip(self.sems, self.counts)) if c]


class T:
    def __init__(self, ap=None, name=""):
        self.ap = ap
        self.name = name
        self.w = None
        self.r = {}


def _deps(q, reads, writes):
    for t in reads:
        q.wait(t.w)
    for t in writes:
        q.wait(t.w)
        for r in t.r.values():
            q.wait(r)


def _commit(tok, reads, writes):
    for t in reads:
        t.r[tok[2]] = tok
    for t in writes:
        t.w = tok
        t.r = {}


def op(q, fn, reads=(), writes=()):
    _deps(q, reads, writes)
    tok = q.issue(fn())
    _commit(tok, reads, writes)
    return tok


def mm(q, fns, reads=(), writes=()):
    _deps(q, reads, writes)
    ins = None
    for f in fns:
        ins = f()
    tok = q.issue(ins)
    _commit(tok, reads, writes)
    return tok


def dma(q, fn, reads=(), writes=()):
    _deps(q, reads, writes)
    tok = q.issue(fn)
    _commit(tok, reads, writes)
    return tok


def build(NSEQ=4, C=1280, debug=False, stop_after=5):
    NT = NSEQ * SEQ
    NCH = NT // 512
    NTT = NT // 128
    NSLOT = NEXP * C
    nc = bass.Bass("TRN2", target_bir_lowering=False)

    def din(name, shape, dt=F32):
        return nc.dram_tensor(name, shape, dt, kind="ExternalInput").ap()

    def dscr(name, shape, dt):
        return nc.dram_tensor(name, shape, dt, kind=("ExternalOutput" if debug else "Internal")).ap()

    x = din("x", [NT, D])
    w_inp = din("w_inp", [D, 5888])
    w_a = din("w_a", [D, D])
    w_b = din("w_b", [256, D])
    w_o = din("w_o", [D, D])
    sinks = din("sinks", [1, 16])
    ln1_g = din("ln1_g", [1, D]); ln1_b = din("ln1_b", [1, D])
    ln2_g = din("ln2_g", [1, D]); ln2_b = din("ln2_b", [1, D])
    w_r = din("w_r", [D, NEXP]); b_r = din("b_r", [1, NEXP])
    NEW = NEXP if stop_after >= 4 else 1
    w_gu = din("w_gu", [NEW, D, 2 * D]); b_gu = din("b_gu", [NEXP, 2 * D])
    w_d = din("w_d", [NEW, D, D]); b_d = din("b_d", [NEXP, D])
    cos_t = din("cos_t", [128, SEQ]); sin_t = din("sin_t", [128, SEQ])
    masks_in = din("masks", [128, 3, 512])
    hm_in = din("hm", [128, 4])
    tri_in = din("tri", [128, 128]); ident_in = din("ident", [128, 128]); ec_in = din("ec", [128, NEXP])
    out = nc.dram_tensor("out", [NT, D], F32, kind="ExternalOutput").ap()

    xb = dscr("xb", [NT, D], BF16)
    qk_scr = dscr("qk_scr", [NQK, 128, NT], BF16)
    v_scr = dscr("v_scr", [NT, 16, 128], BF16)
    oT_scr = dscr("oT_scr", [1280, NT], BF16)
    h1_scr = dscr("h1_scr", [NT, D], F32)
    xg_scr = dscr("xg_scr", [NSLOT, D], BF16)
    y_scr = dscr("y_scr", [NSLOT, D], F32)

    with ExitStack() as es:
        pe = Eng(nc, nc.tensor, "pe", es)
        act = Eng(nc, nc.scalar, "act", es)
        dve = Eng(nc, nc.vector, "dve", es)
        pool = Eng(nc, nc.gpsimd, "pool", es)
        sq = DmaQ(nc, nc.sync, "sq", es, 8)
        gq = DmaQ(nc, nc.gpsimd, "gq", es, 8)
        allq = [pe, act, dve, pool, sq, gq]

        def barrier():
            toks = []
            for q in allq:
                toks += q.alltoks()
            for q in allq:
                for t in toks:
                    q.wait(t)

        def sbt(st, name, shape, dt):
            return T(st.enter_context(nc.sbuf_tensor("sb_" + name, shape, dt)), name)

        def pst(st, name, shape, dt=F32):
            return T(st.enter_context(nc.psum_tensor("ps_" + name, shape, dt)), name)

        bc_reg = nc.gpsimd.to_reg(NSLOT - 1)
        XB = T(xb); QK = T(qk_scr); VS = T(v_scr); OT = T(oT_scr); H1 = T(h1_scr); XG = T(xg_scr); YS = T(y_scr)

        cst = es
        ident = sbt(cst, "ident", [128, 128], F32)
        tri = sbt(cst, "tri", [128, 128], BF16)
        onesb = sbt(cst, "onesb", [128, 128], BF16)
        ec = sbt(cst, "ec", [128, NEXP], F32)
        slots_all = sbt(cst, "slots_all", [128, NTT, 4], I32)
        wts_all = sbt(cst, "wts_all", [128, NTT, 4], F32)
        dma(sq, lambda: nc.sync.dma_start(out=ident.ap[:, :], in_=ident_in[:, :]), writes=[ident])
        dma(gq, lambda: nc.gpsimd.dma_start(out=tri.ap[:, :], in_=tri_in[:, :]), writes=[tri])
        dma(sq, lambda: nc.sync.dma_start(out=ec.ap[:, :], in_=ec_in[:, :]), writes=[ec])
        op(pool, lambda: nc.gpsimd.memset(onesb.ap[:, :], 1.0), writes=[onesb])

        XBs = [T(xb) for _ in range(NT // 1024)]

        def cast_x():
            for i in range(NT // 1024):
                dma(gq, lambda i=i: nc.gpsimd.dma_start(out=xb[i * 1024:(i + 1) * 1024, :], in_=x[i * 1024:(i + 1) * 1024, :]), writes=[XBs[i]])

        with ExitStack() as p1:
            wqkv = sbt(p1, "wqkv", [128, 8, 3840], BF16)
            cosb = sbt(p1, "cosb", [128, SEQ], F32)
            sinb = sbt(p1, "sinb", [128, SEQ], F32)
            xT = [sbt(p1, "xT%d" % i, [128, 8, 512], BF16) for i in range(2)]
            pA = [pst(p1, "pA%d" % i, [128, 512]) for i in range(2)]
            pB = [pst(p1, "pB%d" % i, [128, 512]) for i in range(2)]
            pV = [pst(p1, "pV%d" % i, [128, 512]) for i in range(2)]
            t1 = [sbt(p1, "t1_%d" % i, [128, 512], F32) for i in range(2)]
            t2 = [sbt(p1, "t2_%d" % i, [128, 512], F32) for i in range(2)]
            t3 = [sbt(p1, "t3_%d" % i, [128, 512], F32) for i in range(2)]
            t4 = [sbt(p1, "t4_%d" % i, [128, 512], F32) for i in range(2)]
            qo = [sbt(p1, "qo%d" % i, [128, 2, 512], BF16) for i in range(2)]
            vsb = [sbt(p1, "vsb%d" % i, [128, 16, 128], BF16) for i in range(2)]
            wv = w_inp.rearrange("(c p) n -> p c n", p=128)
            for c0 in range(0, 3840, 1280):
                dma(gq, lambda c0=c0: nc.gpsimd.dma_start(out=wqkv.ap[:, :, c0:c0 + 1280], in_=wv[:, :, c0:c0 + 1280]), writes=[wqkv])
            cast_x()
            dma(sq, lambda: nc.sync.dma_start(out=cosb.ap[:, :], in_=cos_t[:, :]), writes=[cosb])
            dma(sq, lambda: nc.sync.dma_start(out=sinb.ap[:, :], in_=sin_t[:, :]), writes=[sinb])
            for i in range(2):
                op(pool, lambda i=i: nc.gpsimd.memset(vsb[i].ap[:, :, :], 1.0), writes=[vsb[i]])
            zt = sbt(p1, "zt", [128, 8, D], BF16)
            op(pool, lambda: nc.gpsimd.memset(zt.ap[:, :, :], 0.0), writes=[zt])
            for r0_ in range(0, NSLOT, 1024):
                n_ = min(1024, NSLOT - r0_) // 128
                dma(gq, lambda r0_=r0_, n_=n_: nc.gpsimd.dma_start(out=xg_scr[r0_:r0_ + 128 * n_, :].rearrange("(p k) d -> p k d", k=n_), in_=zt.ap[:, 0:n_, :]),
                    reads=[zt], writes=[XG])
            npair = 0
            nv = 0

            def load_xT(ch_):
                for c in range(8):
                    dma(sq, lambda c=c: nc.sync.dma_start_transpose(out=xT[ch_ % 2].ap[:, c, :], in_=xb[ch_ * 512:ch_ * 512 + 512, c * 128:(c + 1) * 128]),
                        reads=[XBs[ch_ // 2]], writes=[xT[ch_ % 2]])

            for ch in range(NCH):
                tok0 = ch * 512
                pos0 = tok0 % SEQ
                xt = xT[ch % 2]
                if ch == 0:
                    load_xT(0)
                if ch + 1 < NCH:
                    load_xT(ch + 1)
                for pr in range(NQK // 2):
                    a, b = pA[npair % 2], pB[npair % 2]
                    u1, u2, u3, u4, o = t1[npair % 2], t2[npair % 2], t3[npair % 2], t4[npair % 2], qo[npair % 2]
                    npair += 1
                    for (pt, ti) in ((a, 2 * pr), (b, 2 * pr + 1)):
                        mm(pe, [lambda c=c, pt=pt, ti=ti: nc.tensor.matmul(pt.ap[:, :], lhsT=wqkv.ap[:, c, ti * 128:(ti + 1) * 128], rhs=xt.ap[:, c, :],
                                                                           start=(c == 0), stop=(c == 7)) for c in range(8)],
                           reads=[wqkv, xt], writes=[pt])
                    cs = cosb.ap[:, pos0:pos0 + 512]
                    sn = sinb.ap[:, pos0:pos0 + 512]
                    op(dve, lambda: nc.vector.tensor_tensor(out=u1.ap[:, :], in0=a.ap[:, :], in1=cs, op=ALU.mult), reads=[a, cosb], writes=[u1])
                    op(dve, lambda: nc.vector.tensor_tensor(out=u2.ap[:, :], in0=b.ap[:, :], in1=sn, op=ALU.mult), reads=[b, sinb], writes=[u2])
                    op(dve, lambda: nc.vector.tensor_tensor(out=u3.ap[:, :], in0=b.ap[:, :], in1=cs, op=ALU.mult), reads=[b, cosb], writes=[u3])
                    op(dve, lambda: nc.vector.tensor_tensor(out=u4.ap[:, :], in0=a.ap[:, :], in1=sn, op=ALU.mult), reads=[a, sinb], writes=[u4])
                    op(pool, lambda: nc.gpsimd.tensor_tensor(out=o.ap[:, 0, :], in0=u1.ap[:, :], in1=u2.ap[:, :], op=ALU.subtract), reads=[u1, u2], writes=[o])
                    op(pool, lambda: nc.gpsimd.tensor_tensor(out=o.ap[:, 1, :], in0=u3.ap[:, :], in1=u4.ap[:, :], op=ALU.add), reads=[u3, u4], writes=[o])
                    dma(sq, lambda: nc.sync.dma_start(out=qk_scr[2 * pr:2 * pr + 2, :, tok0:tok0 + 512].rearrange("t p n -> p t n"), in_=o.ap[:, :, :]),
                        reads=[o], writes=[QK])
                for tt in range(4):
                    vb = vsb[nv % 2]
                    nv += 1
                    for hf in range(2):
                        pv = pV[hf]
                        mm(pe, [lambda c=c, pv=pv, hf=hf: nc.tensor.matmul(pv.ap[:, :], lhsT=xt.ap[:, c, tt * 128:(tt + 1) * 128],
                                                                           rhs=wqkv.ap[:, c, 2816 + hf * 512:2816 + (hf + 1) * 512],
                                                                           start=(c == 0), stop=(c == 7)) for c in range(8)],
                           reads=[wqkv, xt], writes=[pv])
                        op(act, lambda pv=pv, hf=hf, vb=vb: nc.scalar.copy(out=vb.ap[:, hf * 8:(hf + 1) * 8, 0:64],
                                                                           in_=pv.ap[:, :].rearrange("p (h d) -> p h d", d=64)),
                           reads=[pv], writes=[vb])
                    dma(sq, lambda vb=vb, tt=tt: nc.sync.dma_start(out=v_scr[tok0 + tt * 128:tok0 + (tt + 1) * 128, :, :], in_=vb.ap[:, :, :]),
                        reads=[vb], writes=[VS])
            barrier()
        if stop_after <= 1:
            return nc

        with ExitStack() as p2:
            qk = sbt(p2, "qk", [128, 12, SEQ], BF16)
            vA = sbt(p2, "vA", [128, 16, 4, 128], BF16)
            kz = sbt(p2, "kz", [128, 4, 2, SEQ], BF16)
            hm = sbt(p2, "hm", [128, 4], F32)
            vB1 = sbt(p2, "vB", [128, 16, 4, 128], BF16)
            vB = [vB1, vB1, vB1]
            acc = sbt(p2, "acc", [128, 4, SEQ], F32)
            mk = sbt(p2, "mk", [128, 3, 512], BF16)
            identb = sbt(p2, "identb", [128, 128], BF16)
            sk = sbt(p2, "sk", [128, 16], F32)
            ske = sbt(p2, "ske", [128, 16], F32)
            zer = sbt(p2, "zer", [128, 128], F32)
            sink512 = sbt(p2, "sink512", [128, 4, 512], F32)
            pS = [[pst(p2, "pS%d_%d" % (i, kb), [128, 512]) for kb in range(2)] for i in range(2)]
            pO = [pst(p2, "pO%d" % i, [128, 512]) for i in range(2)]
            P = [[sbt(p2, "P%d_%d" % (i, kb), [128, 512], BF16) for kb in range(2)] for i in range(2)]
            tl = [sbt(p2, "tl%d" % i, [128, 512], F32) for i in range(2)]
            rl = [sbt(p2, "rl%d" % i, [64, 512], F32) for i in range(2)]
            oA = [sbt(p2, "oA%d" % i, [64, 4, 512], BF16) for i in range(2)]
            rlB = sbt(p2, "rlB", [64, 4, 128], F32)
            oB = sbt(p2, "oB", [64, 4, 512], BF16)

            dma(gq, lambda: nc.gpsimd.dma_start(out=mk.ap[:, :, :], in_=masks_in[:, :, :]), writes=[mk])
            dma(gq, lambda: nc.gpsimd.dma_start(out=identb.ap[:, :], in_=ident_in[:, :]), writes=[identb])
            dma(sq, lambda: nc.sync.dma_start(out=hm.ap[:, :], in_=hm_in[:, :]), writes=[hm])
            dma(sq, lambda: nc.sync.dma_start(out=sk.ap[:, :], in_=sinks[0:1, :].partition_broadcast(128)), writes=[sk])
            op(act, lambda: nc.scalar.activation(out=ske.ap[:, :], in_=sk.ap[:, :], func=AF.Exp), reads=[sk], writes=[ske])
            op(pool, lambda: nc.gpsimd.memset(zer.ap[:, :], 0.0), writes=[zer])
            for j in range(4):
                for t in range(4):
                    h = 4 * j + t
                    op(dve, lambda j=j, t=t, h=h: nc.vector.tensor_scalar(out=sink512.ap[:, j, t * 128:(t + 1) * 128], in0=zer.ap[:, :],
                                                                          scalar1=ske.ap[:, h:h + 1], scalar2=None, op0=ALU.add),
                       reads=[zer, ske], writes=[sink512])
            MK_CUR, MK_PA, MK_PB = 0, 1, 2
            nu = 0
            for s in range(NSEQ):
                s0 = s * SEQ
                for t_ in range(10):
                    dma(sq, lambda t_=t_: nc.sync.dma_start(out=qk.ap[:, t_, :], in_=qk_scr[t_, :, s0:s0 + SEQ]), reads=[QK], writes=[qk])
                for part in range(2):
                    for hh_ in range(4):
                        op(dve, lambda part=part, hh_=hh_: nc.vector.tensor_scalar(out=kz.ap[:, hh_, part, :], in0=qk.ap[:, T_AK + part, :], scalar1=hm.ap[:, hh_:hh_ + 1], scalar2=None, op0=ALU.mult),
                           reads=[qk, hm], writes=[kz])
                vs = v_scr[s0:s0 + SEQ, :, :]
                dma(sq, lambda: nc.sync.dma_start(out=vA.ap[:, :, :, :], in_=vs[:, 0:4, :].rearrange("(b p) h d -> p b h d", p=128)), reads=[VS], writes=[vA])

                def run_units(units):
                    prev = None
                    for (p1, p2) in units:
                        st = p1()
                        if prev is not None:
                            prev[0](prev[1])
                        prev = (p2, st)
                    if prev is not None:
                        prev[0](prev[1])

                def unit_qk(qsl, ksl, ksl_prev, mprev, is_a, j):
                    nonlocal nu
                    i = nu % 2
                    nu += 1
                    kbs = ([(0, ksl_prev, mprev)] if ksl_prev is not None else []) + [(1, ksl, MK_CUR)]
                    for (kb, ks, mkind) in kbs:
                        S = pS[i][kb]
                        fns = []
                        if is_a:
                            fns.append(lambda S=S, mkind=mkind: nc.tensor.matmul(S.ap[:, :], lhsT=identb.ap[:, :], rhs=mk.ap[:, mkind, :], start=True, stop=False))
                            for part in range(2):
                                fns.append(lambda part=part, ks=ks, S=S: nc.tensor.matmul(
                                    S.ap[:, :], lhsT=kz.ap[:, j, part, ks],
                                    rhs=qk.ap[:, T_AQ + part:T_AQ + 8:2, qsl], start=False, stop=(part == 1)))
                        else:
                            g = j[0]
                            for hh in range(4):
                                fns.append(lambda S=S, mkind=mkind, hh=hh: nc.tensor.matmul(S.ap[:, hh * 128:(hh + 1) * 128], lhsT=identb.ap[:, :], rhs=mk.ap[:, mkind, 0:128],
                                                                                            start=True, stop=False))
                                for part in range(2):
                                    fns.append(lambda part=part, hh=hh, ks=ks, S=S, g=g: nc.tensor.matmul(
                                        S.ap[:, hh * 128:(hh + 1) * 128], lhsT=kz.ap[:, hh, part, ks],
                                        rhs=qk.ap[:, T_BQ + 2 * g + part, qsl], start=False, stop=(part == 1)))
                        mm(pe, fns, reads=[qk, kz, mk, identb], writes=[S])
                        Pt = P[i][kb]
                        op(act, lambda S=S, Pt=Pt: nc.scalar.activation(out=Pt.ap[:, :], in_=S.ap[:, :], func=AF.Exp, scale=0.125), reads=[S], writes=[Pt])
                    return (i, kbs)

                def unit_pv(state, vfn, is_a, j):
                    i, kbs = state
                    O = pO[i]
                    fns = []
                    if is_a:
                        for n_, (kb, ks, mkind) in enumerate(kbs):
                            fns.append(lambda kb=kb, n_=n_: nc.tensor.matmul(O.ap[:, :], lhsT=vfn(kb, 0), rhs=P[i][kb].ap[:, :],
                                                                             start=(n_ == 0), stop=(n_ == len(kbs) - 1)))
                    else:
                        for hh in range(4):
                            for n_, (kb, ks, mkind) in enumerate(kbs):
                                fns.append(lambda kb=kb, n_=n_, hh=hh: nc.tensor.matmul(O.ap[:, hh * 128:(hh + 1) * 128], lhsT=vfn(kb, hh),
                                                                                        rhs=P[i][kb].ap[:, hh * 128:(hh + 1) * 128],
                                                                                        start=(n_ == 0), stop=(n_ == len(kbs) - 1)))
                    mm(pe, fns, reads=[P[i][kb] for (kb, _, _) in kbs] + [vA if is_a else vB[j[0]]], writes=[O])
                    return O, i

                unitsA = []
                for j in [int(c_) for c_ in os.environ.get("P2_A_J", "0123")]:
                    for bg in range(4):
                        ob = oA[(j * 4 + bg) % 2]
                        for bb in range(4):
                            b = bg * 4 + bb
                            qsl = slice(b * 128, (b + 1) * 128)
                            kprev = slice((b - 1) * 128, b * 128) if b > 0 else None

                            def p1(qsl=qsl, kprev=kprev, j=j):
                                return unit_qk(qsl, qsl, kprev, MK_PA, True, j)

                            def p2(state, b=b, j=j, bb=bb, bg=bg, ob=ob):
                                O, i = unit_pv(state, lambda kb, hh: vA.ap[:, b - 1 + kb, j, :], True, j)
                                op(dve, lambda: nc.vector.tensor_tensor(out=tl[i].ap[64:128, :], in0=O.ap[64:128, :], in1=sink512.ap[64:128, j, :], op=ALU.add),
                                   reads=[O, sink512], writes=[tl[i]])
                                op(act, lambda: nc.scalar.activation(out=tl[i].ap[64:128, :], in_=tl[i].ap[64:128, :], func=AF.Ln), reads=[tl[i]], writes=[tl[i]])
                                op(act, lambda: nc.scalar.activation(out=rl[i].ap[:, :], in_=tl[i].ap[64:128, :], func=AF.Exp, scale=-1.0), reads=[tl[i]], writes=[rl[i]])
                                op(dve, lambda: nc.vector.tensor_tensor(out=ob.ap[:, :, bb * 128:(bb + 1) * 128],
                                                                        in0=O.ap[0:64, :].rearrange("p (t n) -> p t n", n=128),
                                                                        in1=rl[i].ap[:, :].rearrange("p (t n) -> p t n", n=128), op=ALU.mult),
                                   reads=[O, rl[i]], writes=[ob])
                                if bb == 3:
                                    dst = oT_scr[256 * j:256 * j + 256, s0 + bg * 512:s0 + (bg + 1) * 512].rearrange("(t d) n -> d t n", d=64)
                                    dma(sq, lambda: nc.sync.dma_start(out=dst, in_=ob.ap[:, :, :]), reads=[ob], writes=[OT])

                            unitsA.append((p1, p2))
                run_units(unitsA)

                for t_ in range(12):
                    dma(sq, lambda t_=t_: nc.sync.dma_start(out=qk.ap[:, t_, :], in_=qk_scr[10 + t_, :, s0:s0 + SEQ]), reads=[QK], writes=[qk])
                for g, (win, dil) in enumerate(B_GROUPS):
                    if str(g) not in os.environ.get("P2_B_G", "012"):
                        continue
                    nblk = 16 // dil
                    for part in range(2):
                        for hh_ in range(4):
                            op(dve, lambda part=part, g=g, hh_=hh_: nc.vector.tensor_scalar(out=kz.ap[:, hh_, part, :], in0=qk.ap[:, T_BK + 2 * g + part, :], scalar1=hm.ap[:, hh_:hh_ + 1], scalar2=None, op0=ALU.mult),
                               reads=[qk, hm], writes=[kz])
                    for r in range(dil):
                        src = vs[:, 4 + 4 * g:8 + 4 * g, :].rearrange("(cb p r) h d -> r p cb h d", p=128, r=dil)[r]
                        dma(sq, lambda g=g, r=r, nblk=nblk, src=src: nc.sync.dma_start(out=vB[g].ap[:, r * nblk:(r + 1) * nblk, :, :], in_=src),
                            reads=[VS], writes=[vB[g]])
                    unitsB = []
                    for r in range(dil):
                        for cb in range(nblk):
                            base = r + dil * 128 * cb
                            qsl = slice(base, base + dil * 127 + 1, dil)
                            kprev = slice(base - dil * 128, base - dil * 128 + dil * 127 + 1, dil) if cb > 0 else None
                            u = r * nblk + cb

                            def p1(qsl=qsl, kprev=kprev, g=g):
                                return unit_qk(qsl, qsl, kprev, MK_PB, False, (g,))

                            def p2(state, qsl=qsl, g=g, u=u):
                                O, i = unit_pv(state, lambda kb, hh: vB[g].ap[:, u - 1 + kb, hh, :], False, (g,))
                                dst = acc.ap[:, :, qsl]
                                src = O.ap[:, :].rearrange("p (h n) -> p h n", n=128)
                                if g == 0:
                                    op(dve, lambda: nc.vector.tensor_copy(out=dst, in_=src), reads=[O], writes=[acc])
                                else:
                                    op(dve, lambda: nc.vector.tensor_tensor(out=dst, in0=src, in1=dst, op=ALU.add), reads=[O, acc], writes=[acc])

                            unitsB.append((p1, p2))
                    run_units(unitsB)
                for bg in range(4):
                    for bb in range(4):
                        csl = slice(bg * 512 + bb * 128, bg * 512 + (bb + 1) * 128)
                        op(act, lambda csl=csl: nc.scalar.activation(out=rlB.ap[:, :, :], in_=acc.ap[64:128, :, csl], func=AF.Ln), reads=[acc], writes=[rlB])
                        op(act, lambda: nc.scalar.activation(out=rlB.ap[:, :, :], in_=rlB.ap[:, :, :], func=AF.Exp, scale=-1.0), reads=[rlB], writes=[rlB])
                        op(dve, lambda csl=csl, bb=bb: nc.vector.tensor_tensor(out=oB.ap[:, :, bb * 128:(bb + 1) * 128], in0=acc.ap[0:64, :, csl], in1=rlB.ap[:, :, :], op=ALU.mult),
                           reads=[acc, rlB], writes=[oB])
                    dst = oT_scr[1024:1280, s0 + bg * 512:s0 + (bg + 1) * 512].rearrange("(t d) n -> d t n", d=64)
                    dma(sq, lambda dst=dst: nc.sync.dma_start(out=dst, in_=oB.ap[:, :, :]), reads=[oB], writes=[OT])
            barrier()
        if stop_after <= 2:
            return nc

        def layer_norm(st_pool, hp, g_t, b_t, outt, tag, scr):
            stats, mv, rstd = scr["stats"], scr["mv"], scr["rstd"]
            for hf in range(2):
                op(dve, lambda hf=hf: nc.vector.bn_stats(out=stats.ap[:, hf, :], in_=hp.ap[:, hf * 512:(hf + 1) * 512]), reads=[hp], writes=[stats])
            op(dve, lambda: nc.vector.bn_aggr(out=mv.ap[:, :], in_=stats.ap[:, :, :].rearrange("p a b -> p (a b)")), reads=[stats], writes=[mv])
            op(dve, lambda: nc.vector.tensor_scalar(out=rstd.ap[:, :], in0=mv.ap[:, 1:2], scalar1=EPS, scalar2=None, op0=ALU.add), reads=[mv], writes=[rstd])
            op(act, lambda: nc.scalar.activation(out=rstd.ap[:, :], in_=rstd.ap[:, :], func=AF.Sqrt), reads=[rstd], writes=[rstd])
            op(dve, lambda: nc.vector.reciprocal(out=rstd.ap[:, :], in_=rstd.ap[:, :]), reads=[rstd], writes=[rstd])
            op(dve, lambda: nc.vector.tensor_scalar(out=outt.ap[:, :], in0=hp.ap[:, :], scalar1=mv.ap[:, 0:1], scalar2=rstd.ap[:, 0:1],
                                                    op0=ALU.subtract, op1=ALU.mult), reads=[hp, mv, rstd], writes=[outt])
            op(dve, lambda: nc.vector.tensor_tensor(out=outt.ap[:, :], in0=outt.ap[:, :], in1=g_t.ap[:, :], op=ALU.mult), reads=[outt, g_t], writes=[outt])
            op(dve, lambda: nc.vector.tensor_tensor(out=outt.ap[:, :], in0=outt.ap[:, :], in1=b_t.ap[:, :], op=ALU.add), reads=[outt, b_t], writes=[outt])

        with ExitStack() as p3:
            wg = sbt(p3, "wg", [128, 8, 2048], BF16)
            wa = sbt(p3, "wa", [128, 8, D], BF16)
            wb = sbt(p3, "wb", [128, 2, D], BF16)
            wo = sbt(p3, "wo", [128, 8, D], BF16)
            wr = sbt(p3, "wr", [128, 8, NEXP], F32)
            brb = sbt(p3, "brb", [128, NEXP], F32)
            g1 = sbt(p3, "g1", [128, D], F32); b1 = sbt(p3, "b1", [128, D], F32)
            xT3 = [sbt(p3, "xT3_%d" % i, [128, 8, 512], BF16) for i in range(2)]
            oT = [sbt(p3, "oT%d" % i, [128, 10, 512], BF16) for i in range(2)]
            mT = [sbt(p3, "mT%d" % i, [128, 8, 512], BF16) for i in range(2)]
            pG = [pst(p3, "pG%d" % i, [128, 512]) for i in range(2)]
            pY = [pst(p3, "pY%d" % i, [128, 512]) for i in range(2)]
            pOut = [pst(p3, "pOut%d" % i, [128, 512]) for i in range(2)]
            pTr = pG
            sg = [sbt(p3, "sg%d" % i, [128, 512], F32) for i in range(2)]
            m1 = sbt(p3, "m1", [128, 512], F32); m2 = sbt(p3, "m2", [128, 512], F32)
            xres = sbt(p3, "xres", [128, D], F32)
            hp = sbt(p3, "hp", [128, D], F32)
            h1s = [sbt(p3, "h1_%d" % i, [128, D], F32) for i in range(4)]
            h1bs = [sbt(p3, "h1b%d" % i, [128, D], BF16) for i in range(4)]
            h1T = [sbt(p3, "h1T%d" % i, [128, 8, 128], F32) for i in range(2)]
            lnscr = {"stats": sbt(p3, "stats", [128, 2, 6], F32), "mv": sbt(p3, "mv", [128, 2], F32), "rstd": sbt(p3, "rstd", [128, 1], F32)}
            pL = pst(p3, "pL", [128, 512])
            pLt = [pL, pL, pL, pL]
            lgs = [sbt(p3, "lg%d" % i, [128, NEXP], F32) for i in range(4)]
            m8s = [sbt(p3, "m8_%d" % i, [128, 8], F32) for i in range(4)]
            nm0 = sbt(p3, "nm0", [128, 1], F32)
            masks_ = [sbt(p3, "mask%d" % i, [128, NEXP], F32) for i in range(4)]
            maskbs = [sbt(p3, "maskb%d" % i, [128, NEXP], BF16) for i in range(4)]
            ee = sbt(p3, "ee", [128, NEXP], F32)
            ssum = sbt(p3, "ssum", [128, 1], F32)
            rs = sbt(p3, "rs", [128, 1], F32)
            W = sbt(p3, "W", [128, NEXP], F32)
            basec = sbt(p3, "basec", [128, NEXP], F32)
            posf = sbt(p3, "posf", [128, NEXP], F32)
            ovf = sbt(p3, "ovf", [128, NEXP], F32)
            oh = sbt(p3, "oh", [128, NEXP], F32)
            junk = sbt(p3, "junk", [128, NEXP], F32)
            slotf = sbt(p3, "slotf", [128, 4], F32)

            wgv = w_inp.rearrange("(c p) n -> p c n", p=128)
            dma(gq, lambda: nc.gpsimd.dma_start(out=wg.ap[:, :, 0:1024], in_=wgv[:, :, 3840:4864]), writes=[wg])
            dma(gq, lambda: nc.gpsimd.dma_start(out=wg.ap[:, :, 1024:2048], in_=wgv[:, :, 4864:5888]), writes=[wg])
            dma(gq, lambda: nc.gpsimd.dma_start(out=wa.ap[:, :, :], in_=w_a.rearrange("(c p) n -> p c n", p=128)), writes=[wa])
            dma(gq, lambda: nc.gpsimd.dma_start(out=wb.ap[:, :, :], in_=w_b.rearrange("(c p) n -> p c n", p=128)), writes=[wb])
            dma(gq, lambda: nc.gpsimd.dma_start(out=wo.ap[:, :, :], in_=w_o.rearrange("(c p) n -> p c n", p=128)), writes=[wo])
            dma(sq, lambda: nc.sync.dma_start(out=wr.ap[:, :, :], in_=w_r.rearrange("(c p) n -> p c n", p=128)), writes=[wr])
            dma(sq, lambda: nc.sync.dma_start(out=brb.ap[:, :], in_=b_r[0:1, :].partition_broadcast(128)), writes=[brb])
            dma(sq, lambda: nc.sync.dma_start(out=g1.ap[:, :], in_=ln1_g[0:1, :].partition_broadcast(128)), writes=[g1])
            dma(sq, lambda: nc.sync.dma_start(out=b1.ap[:, :], in_=ln1_b[0:1, :].partition_broadcast(128)), writes=[b1])
            op(pool, lambda: nc.gpsimd.memset(basec.ap[:, :], 0.0), writes=[basec])

            ng = [0]

            def stage_load(ch):
                tok0 = ch * 512
                xt_, ot_ = xT3[ch % 2], oT[ch % 2]
                for c in range(8):
                    dma(sq, lambda c=c: nc.sync.dma_start_transpose(out=xt_.ap[:, c, :], in_=xb[tok0:tok0 + 512, c * 128:(c + 1) * 128]),
                        reads=[XBs[ch // 2]], writes=[xt_])
                dma(sq, lambda: nc.sync.dma_start(out=ot_.ap[:, :, :], in_=oT_scr[:, tok0:tok0 + 512].rearrange("(c p) n -> p c n", p=128)),
                    reads=[OT], writes=[ot_])

            def stage_gate(ch):
                xt_, ot_, mt_ = xT3[ch % 2], oT[ch % 2], mT[ch % 2]
                for m in range(8):
                    for br in range(2):
                        G = pG[ng[0] % 2]; Y = pY[ng[0] % 2]; S_ = sg[ng[0] % 2]
                        ng[0] += 1
                        col = br * 1024 + m * 128
                        mm(pe, [lambda c=c: nc.tensor.matmul(G.ap[:, :], lhsT=wg.ap[:, c, col:col + 128], rhs=xt_.ap[:, c, :],
                                                             start=(c == 0), stop=(c == 7)) for c in range(8)], reads=[wg, xt_], writes=[G])
                        if br == 0:
                            mm(pe, [lambda c=c: nc.tensor.matmul(Y.ap[:, :], lhsT=wa.ap[:, c, m * 128:(m + 1) * 128], rhs=ot_.ap[:, c, :],
                                                                 start=(c == 0), stop=(c == 7)) for c in range(8)], reads=[wa, ot_], writes=[Y])
                        else:
                            mm(pe, [lambda c=c: nc.tensor.matmul(Y.ap[:, :], lhsT=wb.ap[:, c, m * 128:(m + 1) * 128], rhs=ot_.ap[:, 8 + c, :],
                                                                 start=(c == 0), stop=(c == 1)) for c in range(2)], reads=[wb, ot_], writes=[Y])
                        op(act, lambda: nc.scalar.activation(out=S_.ap[:, :], in_=G.ap[:, :], func=AF.Sigmoid), reads=[G], writes=[S_])
                        mo = m1 if br == 0 else m2
                        op(dve, lambda: nc.vector.tensor_tensor(out=mo.ap[:, :], in0=Y.ap[:, :], in1=S_.ap[:, :], op=ALU.mult),
                           reads=[Y, S_], writes=[mo])
                    op(pool, lambda: nc.gpsimd.tensor_tensor(out=mt_.ap[:, m, :], in0=m1.ap[:, :], in1=m2.ap[:, :], op=ALU.add), reads=[m1, m2], writes=[mt_])

            def stage_out(ch):
                tok0 = ch * 512
                mt_ = mT[ch % 2]
                for tt in range(4):
                    r0 = tok0 + tt * 128
                    h1_, h1b_ = h1s[tt], h1bs[tt]
                    dma(sq, lambda: nc.sync.dma_start(out=xres.ap[:, :], in_=x[r0:r0 + 128, :]), writes=[xres])
                    for hf in range(2):
                        mm(pe, [lambda m=m: nc.tensor.matmul(pOut[hf].ap[:, :], lhsT=mt_.ap[:, m, tt * 128:(tt + 1) * 128], rhs=wo.ap[:, m, hf * 512:(hf + 1) * 512],
                                                             start=(m == 0), stop=(m == 7)) for m in range(8)], reads=[mt_, wo], writes=[pOut[hf]])
                        op(dve, lambda: nc.vector.scalar_tensor_tensor(out=hp.ap[:, hf * 512:(hf + 1) * 512], in0=xres.ap[:, hf * 512:(hf + 1) * 512], scalar=ALPHA,
                                                                       in1=pOut[hf].ap[:, :], op0=ALU.mult, op1=ALU.add), reads=[xres, pOut[hf]], writes=[hp])
                    layer_norm(p3, hp, g1, b1, h1_, "ln1", lnscr)
                    dma(sq, lambda: nc.sync.dma_start(out=h1_scr[r0:r0 + 128, :], in_=h1_.ap[:, :]), reads=[h1_], writes=[H1])
                    op(act, lambda: nc.scalar.copy(out=h1b_.ap[:, :], in_=h1_.ap[:, :]), reads=[h1_], writes=[h1b_])

            def stage_route(ch):
                for tt in range(4):
                    h1_ = h1s[tt]
                    lg, m8, mask, maskb = lgs[tt], m8s[tt], masks_[tt], maskbs[tt]
                    hT_ = h1T[tt % 2]
                    for hf in range(2):
                        mm(pe, [lambda c=c: nc.tensor.transpose(pTr[hf].ap[:, (c % 4) * 128:(c % 4 + 1) * 128], h1_.ap[:, c * 128:(c + 1) * 128], ident.ap[:, :])
                                for c in range(hf * 4, hf * 4 + 4)], reads=[h1_, ident], writes=[pTr[hf]])
                        op(act, lambda: nc.scalar.copy(out=hT_.ap[:, hf * 4:(hf + 1) * 4, :], in_=pTr[hf].ap[:, :].rearrange("p (c n) -> p c n", n=128)),
                           reads=[pTr[hf]], writes=[hT_])
                    L0 = tt * 128
                    mm(pe, [lambda c=c: nc.tensor.matmul(pL.ap[:, L0:L0 + NEXP], lhsT=hT_.ap[:, c, :], rhs=wr.ap[:, c, :], start=(c == 0), stop=(c == 7)) for c in range(8)],
                       reads=[hT_, wr], writes=[pLt[tt]])
                    op(dve, lambda: nc.vector.tensor_tensor(out=lg.ap[:, :], in0=pL.ap[:, L0:L0 + NEXP], in1=brb.ap[:, :], op=ALU.add), reads=[pLt[tt], brb], writes=[lg])
                    op(dve, lambda: nc.vector.max(out=m8.ap[:, :], in_=lg.ap[:, :]), reads=[lg], writes=[m8])
                    op(dve, lambda: nc.vector.tensor_scalar(out=mask.ap[:, :], in0=lg.ap[:, :], scalar1=m8.ap[:, 3:4], scalar2=None, op0=ALU.is_ge), reads=[lg, m8], writes=[mask])
                    op(dve, lambda: nc.vector.tensor_copy(out=maskb.ap[:, :], in_=mask.ap[:, :]), reads=[mask], writes=[maskb])
                for tt in range(4):
                    ti = ch * 4 + tt
                    h1b_ = h1bs[tt]
                    lg, m8, mask, maskb = lgs[tt], m8s[tt], masks_[tt], maskbs[tt]
                    L0 = tt * 128
                    op(dve, lambda: nc.vector.tensor_scalar(out=nm0.ap[:, :], in0=m8.ap[:, 0:1], scalar1=-1.0, scalar2=None, op0=ALU.mult), reads=[m8], writes=[nm0])
                    op(act, lambda: nc.scalar.activation(out=ee.ap[:, :], in_=lg.ap[:, :], func=AF.Exp, bias=nm0.ap[:, 0:1], scale=1.0), reads=[lg, nm0], writes=[ee])
                    op(dve, lambda: nc.vector.tensor_tensor(out=ee.ap[:, :], in0=ee.ap[:, :], in1=mask.ap[:, :], op=ALU.mult), reads=[ee, mask], writes=[ee])
                    op(dve, lambda: nc.vector.reduce_sum(out=ssum.ap[:, :], in_=ee.ap[:, :], axis=mybir.AxisListType.X), reads=[ee], writes=[ssum])
                    op(dve, lambda: nc.vector.reciprocal(out=rs.ap[:, :], in_=ssum.ap[:, :]), reads=[ssum], writes=[rs])
                    op(dve, lambda: nc.vector.tensor_scalar(out=W.ap[:, :], in0=ee.ap[:, :], scalar1=rs.ap[:, 0:1], scalar2=None, op0=ALU.mult), reads=[ee, rs], writes=[W])
                    mm(pe, [lambda: nc.tensor.matmul(pL.ap[:, L0 + 32:L0 + 64], lhsT=tri.ap[:, :], rhs=maskb.ap[:, :], start=True, stop=True),
                            lambda: nc.tensor.matmul(pL.ap[:, L0 + 64:L0 + 96], lhsT=onesb.ap[:, :], rhs=maskb.ap[:, :], start=True, stop=True)],
                       reads=[tri, onesb, maskb], writes=[pLt[tt]])
                    op(dve, lambda: nc.vector.tensor_tensor(out=posf.ap[:, :], in0=pL.ap[:, L0 + 32:L0 + 64], in1=basec.ap[:, :], op=ALU.add), reads=[pLt[tt], basec], writes=[posf])
                    op(dve, lambda: nc.vector.tensor_tensor(out=basec.ap[:, :], in0=pL.ap[:, L0 + 64:L0 + 96], in1=basec.ap[:, :], op=ALU.add), reads=[pLt[tt], basec], writes=[basec])
                    op(dve, lambda: nc.vector.tensor_scalar(out=ovf.ap[:, :], in0=posf.ap[:, :], scalar1=float(C), scalar2=None, op0=ALU.is_lt), reads=[posf], writes=[ovf])
                    op(dve, lambda: nc.vector.tensor_tensor(out=W.ap[:, :], in0=W.ap[:, :], in1=ovf.ap[:, :], op=ALU.mult), reads=[W, ovf], writes=[W])
                    op(dve, lambda: nc.vector.tensor_scalar(out=ovf.ap[:, :], in0=ovf.ap[:, :], scalar1=-1.0, scalar2=-4.0e6, op0=ALU.add, op1=ALU.mult), reads=[ovf], writes=[ovf])
                    op(dve, lambda: nc.vector.tensor_tensor(out=posf.ap[:, :], in0=posf.ap[:, :], in1=ec.ap[:, :], op=ALU.add), reads=[posf, ec], writes=[posf])
                    op(dve, lambda: nc.vector.tensor_tensor(out=posf.ap[:, :], in0=posf.ap[:, :], in1=ovf.ap[:, :], op=ALU.add), reads=[posf, ovf], writes=[posf])
                    for k in range(4):
                        op(dve, lambda: nc.vector.tensor_scalar(out=oh.ap[:, :], in0=lg.ap[:, :], scalar1=m8.ap[:, k:k + 1], scalar2=None, op0=ALU.is_equal), reads=[lg, m8], writes=[oh])
                        op(dve, lambda: nc.vector.tensor_tensor(out=junk.ap[:, :], in0=oh.ap[:, :], in1=posf.ap[:, :], op=ALU.mult), reads=[oh, posf], writes=[junk])
                        op(dve, lambda: nc.vector.reduce_sum(out=slotf.ap[:, k:k + 1], in_=junk.ap[:, :], axis=mybir.AxisListType.X), reads=[junk], writes=[slotf])
                        op(dve, lambda: nc.vector.tensor_tensor(out=junk.ap[:, :], in0=oh.ap[:, :], in1=W.ap[:, :], op=ALU.mult), reads=[oh, W], writes=[junk])
                        op(dve, lambda: nc.vector.reduce_sum(out=wts_all.ap[:, ti, k:k + 1], in_=junk.ap[:, :], axis=mybir.AxisListType.X), reads=[junk], writes=[wts_all])
                    op(dve, lambda: nc.vector.tensor_copy(out=slots_all.ap[:, ti, :], in_=slotf.ap[:, :]), reads=[slotf], writes=[slots_all])
                    for k in range(4):
                        dma(gq, lambda: nc.gpsimd.indirect_dma_start(
                            out=xg_scr[:, :], out_offset=bass.IndirectOffsetOnAxis(ap=slots_all.ap[:, ti, k:k + 1], axis=0),
                            in_=h1b_.ap[:, :], in_offset=None, bounds_check=bc_reg, oob_is_err=False),
                            reads=[h1b_, slots_all], writes=[XG])

            stage_load(0)
            stage_gate(0)
            for ch in range(NCH):
                if ch + 1 < NCH:
                    stage_load(ch + 1)
                if ch > 0:
                    stage_route(ch - 1)
                stage_out(ch)
                if ch + 1 < NCH:
                    stage_gate(ch + 1)
            stage_route(NCH - 1)
            barrier()
        if stop_after <= 3:
            return nc

        with ExitStack() as p4:
            wgu = [sbt(p4, "wgu%d" % i, [128, 8, 2 * D], BF16) for i in range(2)]
            wd = [sbt(p4, "wd%d" % i, [128, 8, D], BF16) for i in range(2)]
            bgu = [sbt(p4, "bgu%d" % i, [128, 16], F32) for i in range(2)]
            bdr = [sbt(p4, "bdr%d" % i, [1, D], BF16) for i in range(2)]
            xg = [sbt(p4, "xg%d" % i, [128, 8, 512], BF16) for i in range(2)]
            hT = [sbt(p4, "hT%d" % i, [128, 8, 512], BF16) for i in range(2)]
            pGg = [pst(p4, "pGg%d" % i, [128, 512]) for i in range(2)]
            pUu = [pst(p4, "pUu%d" % i, [128, 512]) for i in range(2)]
            pYy = [pst(p4, "pYy%d" % i, [128, 512]) for i in range(4)]
            gs_ = [sbt(p4, "gsb%d" % i, [128, 512], F32) for i in range(2)]
            ss_ = [sbt(p4, "ssb%d" % i, [128, 512], F32) for i in range(2)]
            us_ = [sbt(p4, "usb%d" % i, [128, 512], F32) for i in range(2)]
            ysb = [sbt(p4, "ysb%d" % i, [128, D], F32) for i in range(2)]
            batches = []
            s_ = 0
            BS = 384 if C % 384 == 0 else 512
            while s_ < C:
                batches.append((s_, min(BS, C - s_)))
                s_ += BS
            nm = 0
            ny = 0
            items = [(e, sb0, S) for e in range(NEXP) for (sb0, S) in batches]

            def load_w(e):
                i2 = e % 2
                for c0 in range(0, 2048, 1024):
                    dma(gq, lambda c0=c0: nc.gpsimd.dma_start(out=wgu[i2].ap[:, :, c0:c0 + 1024], in_=w_gu[e].rearrange("(c p) n -> p c n", p=128)[:, :, c0:c0 + 1024]),
                        writes=[wgu[i2]])
                dma(gq, lambda: nc.gpsimd.dma_start(out=wd[i2].ap[:, :, :], in_=w_d[e].rearrange("(c p) n -> p c n", p=128)), writes=[wd[i2]])
                dma(gq, lambda: nc.gpsimd.dma_start(out=bgu[i2].ap[:, :], in_=b_gu[e].rearrange("(m p) -> p m", p=128), allow_slow_non_contiguous=True), writes=[bgu[i2]])
                dma(gq, lambda: nc.gpsimd.dma_start(out=bdr[i2].ap[:, :], in_=b_d[e:e + 1, :]), writes=[bdr[i2]])

            def load_xg(idx):
                e, sb0, S = items[idx]
                xgt = xg[idx % 2]
                r0 = e * C + sb0
                for c in range(8):
                    dma(sq, lambda c=c: nc.sync.dma_start_transpose(out=xgt.ap[:, c, 0:S], in_=xg_scr[r0:r0 + S, c * 128:(c + 1) * 128]),
                        reads=[XG], writes=[xgt])

            load_w(0)
            load_xg(0)
            for idx, (e, sb0, S) in enumerate(items):
                i2 = e % 2
                if sb0 == 0:
                    op(dve, lambda: nc.vector.tensor_scalar(out=bgu[i2].ap[:, 8:16], in0=bgu[i2].ap[:, 8:16], scalar1=1.0, scalar2=None, op0=ALU.add), reads=[bgu[i2]], writes=[bgu[i2]])
                    if e + 1 < NEXP:
                        load_w(e + 1)
                if idx + 1 < len(items):
                    load_xg(idx + 1)
                xgt = xg[idx % 2]; ht = hT[idx % 2]
                r0 = e * C + sb0
                for m in range(8):
                    Gp = pGg[nm % 2]; Up = pUu[nm % 2]; gsb = gs_[nm % 2]; ssb = ss_[nm % 2]; usb = us_[nm % 2]
                    nm += 1
                    mm(pe, [lambda c=c: nc.tensor.matmul(Gp.ap[:, 0:S], lhsT=wgu[i2].ap[:, c, m * 128:(m + 1) * 128], rhs=xgt.ap[:, c, 0:S],
                                                         start=(c == 0), stop=(c == 7)) for c in range(8)], reads=[wgu[i2], xgt], writes=[Gp])
                    mm(pe, [lambda c=c: nc.tensor.matmul(Up.ap[:, 0:S], lhsT=wgu[i2].ap[:, c, D + m * 128:D + (m + 1) * 128], rhs=xgt.ap[:, c, 0:S],
                                                         start=(c == 0), stop=(c == 7)) for c in range(8)], reads=[wgu[i2], xgt], writes=[Up])
                    op(dve, lambda: nc.vector.tensor_scalar(out=gsb.ap[:, 0:S], in0=Gp.ap[:, 0:S], scalar1=bgu[i2].ap[:, m:m + 1], scalar2=7.0,
                                                            op0=ALU.add, op1=ALU.min), reads=[Gp, bgu[i2]], writes=[gsb])
                    op(act, lambda: nc.scalar.activation(out=ssb.ap[:, 0:S], in_=gsb.ap[:, 0:S], func=AF.Sigmoid, scale=1.702), reads=[gsb], writes=[ssb])
                    op(dve, lambda: nc.vector.tensor_scalar(out=usb.ap[:, 0:S], in0=Up.ap[:, 0:S], scalar1=bgu[i2].ap[:, 8 + m:9 + m], scalar2=8.0,
                                                            op0=ALU.add, op1=ALU.min), reads=[Up, bgu[i2]], writes=[usb])
                    op(pool, lambda: nc.gpsimd.tensor_tensor(out=gsb.ap[:, 0:S], in0=gsb.ap[:, 0:S], in1=ssb.ap[:, 0:S], op=ALU.mult),
                       reads=[gsb, ssb], writes=[gsb])
                    op(dve, lambda: nc.vector.scalar_tensor_tensor(out=ht.ap[:, m, 0:S], in0=usb.ap[:, 0:S], scalar=-6.0, in1=gsb.ap[:, 0:S],
                                                                   op0=ALU.max, op1=ALU.mult), reads=[gsb, usb], writes=[ht])
                for st in range(S // 128):
                    yb = ysb[ny % 2]
                    for hf in range(2):
                        Yp = pYy[(2 * ny + hf) % 4]
                        fns = [lambda m=m: nc.tensor.matmul(Yp.ap[:, :], lhsT=ht.ap[:, m, st * 128:(st + 1) * 128], rhs=wd[i2].ap[:, m, hf * 512:(hf + 1) * 512],
                                                            start=(m == 0), stop=False) for m in range(8)]
                        fns.append(lambda: nc.tensor.matmul(Yp.ap[:, :], lhsT=onesb.ap[0:1, :], rhs=bdr[i2].ap[0:1, hf * 512:(hf + 1) * 512], start=False, stop=True))
                        mm(pe, fns, reads=[ht, wd[i2], bdr[i2], onesb], writes=[Yp])
                        op(act, lambda: nc.scalar.copy(out=yb.ap[:, hf * 512:(hf + 1) * 512], in_=Yp.ap[:, :]), reads=[Yp], writes=[yb])
                    ny += 1
                    rr = r0 + st * 128
                    dma(sq, lambda: nc.sync.dma_start(out=y_scr[rr:rr + 128, :], in_=yb.ap[:, :]), reads=[yb], writes=[YS])
            barrier()
        if stop_after <= 4:
            return nc

        with ExitStack() as p5:
            g2 = sbt(p5, "g2", [128, D], F32); b2 = sbt(p5, "b2", [128, D], F32)
            yk = [[sbt(p5, "yk%d_%d" % (i, k), [128, D], F32) for k in range(4)] for i in range(2)]
            hres = [sbt(p5, "hres%d" % i, [128, D], F32) for i in range(2)]
            acc5 = [sbt(p5, "acc5_%d" % i, [128, D], F32) for i in range(2)]
            o5 = [sbt(p5, "o5_%d" % i, [128, D], F32) for i in range(2)]
            lnscr5 = [{"stats": sbt(p5, "stats5%d" % i, [128, 2, 6], F32), "mv": sbt(p5, "mv5%d" % i, [128, 2], F32), "rstd": sbt(p5, "rstd5%d" % i, [128, 1], F32)} for i in range(2)]
            dma(sq, lambda: nc.sync.dma_start(out=g2.ap[:, :], in_=ln2_g[0:1, :].partition_broadcast(128)), writes=[g2])
            dma(sq, lambda: nc.sync.dma_start(out=b2.ap[:, :], in_=ln2_b[0:1, :].partition_broadcast(128)), writes=[b2])
            for i in range(2):
                for k in range(4):
                    op(pool, lambda i=i, k=k: nc.gpsimd.memset(yk[i][k].ap[:, :], 0.0), writes=[yk[i][k]])
            for ti in range(NTT):
                i = ti % 2
                r0 = ti * 128
                dma(sq, lambda r0=r0, i=i: nc.sync.dma_start(out=hres[i].ap[:, :], in_=h1_scr[r0:r0 + 128, :]), reads=[H1], writes=[hres[i]])
                for k in range(4):
                    dma(gq, lambda k=k, ti=ti, i=i: nc.gpsimd.indirect_dma_start(
                        out=yk[i][k].ap[:, :], out_offset=None, in_=y_scr[:, :],
                        in_offset=bass.IndirectOffsetOnAxis(ap=slots_all.ap[:, ti, k:k + 1], axis=0), bounds_check=bc_reg, oob_is_err=False),
                        reads=[YS, slots_all], writes=[yk[i][k]])
                a5 = acc5[i]
                op(dve, lambda i=i, ti=ti, a5=a5: nc.vector.tensor_scalar(out=a5.ap[:, :], in0=yk[i][0].ap[:, :], scalar1=wts_all.ap[:, ti, 0:1], scalar2=None, op0=ALU.mult),
                   reads=[yk[i][0], wts_all], writes=[a5])
                for k in range(1, 4):
                    eng = dve
                    ee_ = nc.vector
                    op(eng, lambda i=i, ti=ti, k=k, a5=a5, ee_=ee_: ee_.scalar_tensor_tensor(out=a5.ap[:, :], in0=yk[i][k].ap[:, :], scalar=wts_all.ap[:, ti, k:k + 1], in1=a5.ap[:, :],
                                                                                          op0=ALU.mult, op1=ALU.add), reads=[yk[i][k], wts_all, a5], writes=[a5])
                op(dve, lambda i=i, a5=a5: nc.vector.scalar_tensor_tensor(out=a5.ap[:, :], in0=hres[i].ap[:, :], scalar=ALPHA, in1=a5.ap[:, :], op0=ALU.mult, op1=ALU.add),
                   reads=[hres[i], a5], writes=[a5])
                layer_norm(p5, a5, g2, b2, o5[i], "ln2", lnscr5[i])
                dma(sq, lambda r0=r0, i=i: nc.sync.dma_start(out=out[r0:r0 + 128, :], in_=o5[i].ap[:, :]), reads=[o5[i]])
            barrier()
    return nc


def _perm_cols():
    OFF_AQ, OFF_AK, OFF_AV, OFF_BQ, OFF_BK, OFF_BV, OFF_G = 0, 1024, 1280, 1536, 2304, 3072, 3840
    cols = []

    def tile(heads_off, part):
        for off in heads_off:
            cols.extend(range(off + 32 * part, off + 32 * part + 32))

    for t in range(4):
        for part in range(2):
            tile([OFF_AQ + (4 * j + t) * 64 for j in range(4)], part)
    for part in range(2):
        tile([OFF_AK + j * 64 for j in range(4)], part)
    for g in range(3):
        for part in range(2):
            tile([OFF_BQ + (4 * g + j) * 64 for j in range(4)], part)
    for g in range(3):
        for part in range(2):
            tile([OFF_BK + (4 * g + j) * 64 for j in range(4)], part)
    cols.extend(range(OFF_AV, OFF_AV + 256))
    cols.extend(range(OFF_BV, OFF_BV + 768))
    cols.extend(range(OFF_G, OFF_G + 2048))
    return np.asarray(cols, dtype=np.int64)


def _consts(C):
    p = np.arange(128)
    inv_freq = 10000.0 ** (-(np.arange(32, dtype=np.float64)) / 32.0)
    ang = np.arange(SEQ, dtype=np.float64)[None, :] * inv_freq[p % 32][:, None]
    cos_t = np.cos(ang).astype(np.float32)
    sin_t = np.sin(ang).astype(np.float32)
    k = np.arange(128)[:, None]
    q = np.arange(128)[None, :]
    m_cur = (k <= q)
    m_pa = (k >= q + 1)
    m_pb = (k >= q)
    masks = np.stack([np.tile(np.where(m, 0.0, -30000.0), (1, 4)) for m in (m_cur, m_pa, m_pb)], axis=1).astype(np.float32)
    tri = (np.arange(128)[:, None] < np.arange(128)[None, :]).astype(np.float32)
    ident = np.eye(128, dtype=np.float32)
    ec = np.tile((np.arange(NEXP) * C).astype(np.float32)[None, :], (128, 1))
    hm = (np.arange(128)[:, None] // 32 == np.arange(4)[None, :]).astype(np.float32)
    return dict(cos_t=cos_t, sin_t=sin_t, masks=masks, tri=tri, ident=ident, ec=ec, hm=hm)


def make_in_maps(inputs, ncores, NSEQ, C):
    x = np.asarray(inputs["x"], dtype=np.float32)
    perm = _perm_cols()
    w_inp = np.ascontiguousarray(np.asarray(inputs["w_in"])[0][:, perm])
    shared = dict(
        w_inp=w_inp,
        w_a=np.ascontiguousarray(inputs["w_branch_a"][0]), w_b=np.ascontiguousarray(inputs["w_branch_b"][0]),
        w_o=np.ascontiguousarray(inputs["w_out"][0]),
        sinks=np.ascontiguousarray(inputs["attn_sinks"]).reshape(1, 16),
        ln1_g=np.asarray(inputs["ln1_g"]).reshape(1, D), ln1_b=np.asarray(inputs["ln1_b"]).reshape(1, D),
        ln2_g=np.asarray(inputs["ln2_g"]).reshape(1, D), ln2_b=np.asarray(inputs["ln2_b"]).reshape(1, D),
        w_r=np.ascontiguousarray(inputs["w_router"][0]), b_r=np.asarray(inputs["b_router"]).reshape(1, NEXP),
        w_gu=np.ascontiguousarray(inputs["w_gate_up"][0]), b_gu=np.ascontiguousarray(inputs["b_gate_up"][0]),
        w_d=np.ascontiguousarray(inputs["w_down"][0]), b_d=np.ascontiguousarray(inputs["b_down"][0]),
    )
    shared = {k: np.asarray(v, dtype=np.float32) for k, v in shared.items()}
    shared.update(_consts(C))
    maps = []
    for c in range(ncores):
        m = dict(shared)
        m["x"] = np.ascontiguousarray(x[c * NSEQ:(c + 1) * NSEQ].reshape(NSEQ * SEQ, D))
        maps.append(m)
    return maps


def kernel(**inputs):
    ncores, NSEQ, C = 8, 4, 1152
    nc = build(NSEQ=NSEQ, C=C)
    in_maps = make_in_maps(inputs, ncores, NSEQ, C)
    res = run_bass_kernel_spmd(nc, in_maps, core_ids=list(range(ncores)))
    outs = [np.asarray(r["out"], dtype=np.float32).reshape(NSEQ, SEQ, D) for r in res.results]
    return np.concatenate(outs, axis=0)
```
